# Optimizing a Trainium2 kernel written in Bass

```python
import math
import jax, jax.numpy as jnp
from jax import lax
import numpy as np

D_MODEL = 2048
BATCH = 8
SEQ = 2048
DEPTH = 2

A_HEADS = 8
HEAD_DIM = 128
A_WIDTH = A_HEADS * HEAD_DIM
DILATED_BRANCHES = ((128, 1), (512, 4), (2048, 16))
REL_BUCKETS = 32
REL_MAX_DIST = 2048
CONV_CH = D_MODEL - A_WIDTH
CONV_WIDTH = 31
IN_EVEN = 3 * A_WIDTH + 2 * CONV_CH
MIX_EVEN = A_WIDTH + CONV_CH
GMLP_WIDTH = D_MODEL
CHUNK = 128
GMLP_GROUPS = 16
GMLP_GROUP_CH = GMLP_WIDTH // GMLP_GROUPS
FF_DENSE = 5632
N_EXPERTS = 8
TOP_K = 2
FF_EXPERT = 7168
EPS = 1e-6
N_EVEN = (DEPTH + 1) // 2
N_ODD = DEPTH // 2

kernel_name = 'hybrid_dilated_conformer_gmlp_moe'


def _rmsnorm(x, g):
    x32 = x.astype(jnp.float32)
    y = x32 * lax.rsqrt(jnp.mean(x32 * x32, axis=-1, keepdims=True) + EPS)
    return (y * g).astype(x.dtype)


def _layernorm(x, g, b):
    x32 = x.astype(jnp.float32)
    mu = jnp.mean(x32, axis=-1, keepdims=True)
    xc = x32 - mu
    y = xc * lax.rsqrt(jnp.mean(xc * xc, axis=-1, keepdims=True) + EPS)
    return (y * g + b).astype(x.dtype)


def _t5_bucket(dist):
    max_exact = REL_BUCKETS // 2
    d = jnp.maximum(dist, 0)
    log_ratio = jnp.log(jnp.maximum(d, 1).astype(jnp.float32) / max_exact) / math.log(REL_MAX_DIST / max_exact)
    large = max_exact + (log_ratio * (REL_BUCKETS - max_exact)).astype(jnp.int32)
    large = jnp.minimum(large, REL_BUCKETS - 1)
    return jnp.where(d < max_exact, d, large)


def _dilated_branch(q, k, v, rel_bias, window, dil):
    bsz, s, h, e = q.shape
    blk = window // dil
    n_pos = s // dil
    nb = -(-n_pos // blk)
    lp = nb * blk

    def split(t):
        t = t.reshape(bsz, n_pos, dil, h, e)
        t = jnp.pad(t, ((0, 0), (0, lp - n_pos), (0, 0), (0, 0), (0, 0)))
        return t.reshape(bsz, nb, blk, dil, h, e)

    def with_prev(t):
        prev = jnp.pad(t, ((0, 0), (1, 0), (0, 0), (0, 0), (0, 0), (0, 0)))[:, :-1]
        return jnp.concatenate([prev, t], axis=2)

    qb = split(q)
    kb = with_prev(split(k))
    vb = with_prev(split(v))
    logits = jnp.einsum('bnirhe,bnjrhe->bnrhij', qb, kb) * (e ** -0.5)
    qi = jnp.arange(blk)[:, None]
    kj = jnp.arange(2 * blk)[None, :]
    dm = qi + blk - kj
    bias = rel_bias[_t5_bucket(dm * dil)].astype(jnp.float32)
    logits = logits + jnp.transpose(bias, (2, 0, 1))
    in_band = (dm >= 0) & (dm <= blk)
    prev_ok = (jnp.arange(nb)[:, None, None] > 0) | (kj[None] >= blk)
    mask = (in_band[None] & prev_ok)[None, :, None, None]
    logits = jnp.where(mask, logits, -jnp.inf)
    lse = jax.nn.logsumexp(logits, axis=-1)
    p = jnp.exp(logits - lse[..., None])
    o = jnp.einsum('bnrhij,bnjrhe->bnirhe', p, vb)
    o = o.reshape(bsz, lp, dil, h, e)[:, :n_pos].reshape(bsz, s, h, e)
    lse = jnp.transpose(lse, (0, 1, 4, 2, 3)).reshape(bsz, lp, dil, h)[:, :n_pos].reshape(bsz, s, h)
    return o, lse


def _dilated_mixture(q, k, v, rel_bias):
    outs, lses = [], []
    for window, dil in DILATED_BRANCHES:
        o, l = _dilated_branch(q, k, v, rel_bias, window, dil)
        outs.append(o)
        lses.append(l)
    wts = jax.nn.softmax(jnp.stack(lses), axis=0)
    return jnp.sum(wts[..., None] * jnp.stack(outs), axis=0)


def _causal_depthwise_conv(x, w, b):
    kw, c = w.shape
    y = lax.conv_general_dilated(x, w[:, None, :].astype(x.dtype), window_strides=(1,),
                                 padding=[(kw - 1, 0)], dimension_numbers=('NWC', 'WIO', 'NWC'),
                                 feature_group_count=c)
    return y + b


def _even_mixer(h, w_in, q_g, k_g, rel_bias, conv_w, conv_b, cn_g, cn_b, w_out):
    bsz, s, _ = h.shape
    p = h @ w_in
    q, k, v, cv, cg = jnp.split(p, [A_WIDTH, 2 * A_WIDTH, 3 * A_WIDTH, 3 * A_WIDTH + CONV_CH], axis=-1)

    def heads(t):
        return t.reshape(bsz, s, A_HEADS, HEAD_DIM).astype(jnp.float32)

    q = _rmsnorm(heads(q), q_g)
    k = _rmsnorm(heads(k), k_g)
    o_a = _dilated_mixture(q, k, heads(v), rel_bias).reshape(bsz, s, A_WIDTH).astype(h.dtype)
    c = cv * jax.nn.sigmoid(cg)
    c = _causal_depthwise_conv(c, conv_w, conv_b)
    o_b = jax.nn.silu(_layernorm(c, cn_g, cn_b))
    return jnp.concatenate([o_a, o_b], axis=-1) @ w_out


def _gmlp_mixer(h, w_u, b_u, vn_g, vn_b, w_s, b_s, w_o):
    bsz, s, _ = h.shape
    z = jax.nn.gelu(h @ w_u + b_u)
    u, v = jnp.split(z, 2, axis=-1)
    v = _layernorm(v, vn_g, vn_b).reshape(bsz, s // CHUNK, CHUNK, GMLP_GROUPS, GMLP_GROUP_CH)
    causal = jnp.tril(jnp.ones((CHUNK, CHUNK), dtype=bool))
    ws = jnp.where(causal[None], w_s, jnp.zeros((), w_s.dtype)).astype(v.dtype)
    sv = jnp.einsum('gts,bcsgd->bctgd', ws, v) + b_s.T[:, :, None]
    return (u * sv.reshape(bsz, s, GMLP_WIDTH)) @ w_o


def _swiglu(h, w1, w3, w2):
    return (jax.nn.silu(h @ w1) * (h @ w3)) @ w2


def _moe_swiglu(h, w_router, w_gate, w_up, w_down):
    bsz, s, d = h.shape
    t = h.reshape(-1, d)
    n = t.shape[0]
    logits = t.astype(jnp.float32) @ w_router.astype(jnp.float32)
    top_val, top_idx = lax.top_k(logits, TOP_K)
    gates = jax.nn.softmax(top_val, axis=-1)
    expert = top_idx.reshape(-1)
    token = jnp.repeat(jnp.arange(n, dtype=jnp.int32), TOP_K)
    order = jnp.argsort(expert)
    tok_s = token[order]
    group_sizes = jnp.bincount(expert, length=N_EXPERTS).astype(jnp.int32)
    xs = t[tok_s]
    hid = jax.nn.silu(lax.ragged_dot(xs, w_gate, group_sizes)) * lax.ragged_dot(xs, w_up, group_sizes)
    ys = lax.ragged_dot(hid, w_down, group_sizes)
    ys = ys * gates.reshape(-1)[order][:, None].astype(ys.dtype)
    out = jnp.zeros_like(t).at[tok_s].add(ys)
    return out.reshape(bsz, s, d)


def setup_inputs(seed: int = 0) -> dict:
    key = jax.random.key(seed)
    ks = iter(jax.random.split(key, 32))

    def nrm(shape, scale):
        return jax.random.normal(next(ks), shape, jnp.float32) * scale

    def gain(shape):
        return 1.0 + nrm(shape, 0.02)

    return {
        'x': nrm((BATCH, SEQ, D_MODEL), 1.0),
        'rel_bias': nrm((REL_BUCKETS, A_HEADS), 0.1),
        'even_norm_mix': gain((N_EVEN, D_MODEL)),
        'even_w_in': nrm((N_EVEN, D_MODEL, IN_EVEN), D_MODEL ** -0.5),
        'even_q_norm': gain((N_EVEN, HEAD_DIM)),
        'even_k_norm': gain((N_EVEN, HEAD_DIM)),
        'even_conv_w': nrm((N_EVEN, CONV_WIDTH, CONV_CH), CONV_WIDTH ** -0.5),
        'even_conv_b': nrm((N_EVEN, CONV_CH), 0.02),
        'even_cnorm_g': gain((N_EVEN, CONV_CH)),
        'even_cnorm_b': nrm((N_EVEN, CONV_CH), 0.02),
        'even_w_out': nrm((N_EVEN, MIX_EVEN, D_MODEL), MIX_EVEN ** -0.5),
        'even_norm_ffn': gain((N_EVEN, D_MODEL)),
        'even_ffn_w1': nrm((N_EVEN, D_MODEL, FF_DENSE), D_MODEL ** -0.5),
        'even_ffn_w3': nrm((N_EVEN, D_MODEL, FF_DENSE), D_MODEL ** -0.5),
        'even_ffn_w2': nrm((N_EVEN, FF_DENSE, D_MODEL), FF_DENSE ** -0.5),
        'odd_norm_mix': gain((N_ODD, D_MODEL)),
        'odd_w_u': nrm((N_ODD, D_MODEL, 2 * GMLP_WIDTH), D_MODEL ** -0.5),
        'odd_b_u': nrm((N_ODD, 2 * GMLP_WIDTH), 0.02),
        'odd_vnorm_g': gain((N_ODD, GMLP_WIDTH)),
        'odd_vnorm_b': nrm((N_ODD, GMLP_WIDTH), 0.02),
        'odd_w_s': nrm((N_ODD, GMLP_GROUPS, CHUNK, CHUNK), CHUNK ** -0.5),
        'odd_b_s': nrm((N_ODD, GMLP_GROUPS, CHUNK), 0.02),
        'odd_w_o': nrm((N_ODD, GMLP_WIDTH, D_MODEL), GMLP_WIDTH ** -0.5),
        'odd_norm_ffn': gain((N_ODD, D_MODEL)),
        'odd_router': nrm((N_ODD, D_MODEL, N_EXPERTS), D_MODEL ** -0.5),
        'odd_we_gate': nrm((N_ODD, N_EXPERTS, D_MODEL, FF_EXPERT), D_MODEL ** -0.5),
        'odd_we_up': nrm((N_ODD, N_EXPERTS, D_MODEL, FF_EXPERT), D_MODEL ** -0.5),
        'odd_we_down': nrm((N_ODD, N_EXPERTS, FF_EXPERT, D_MODEL), FF_EXPERT ** -0.5),
    }


def reference(x, rel_bias, even_norm_mix, even_w_in, even_q_norm, even_k_norm, even_conv_w,
              even_conv_b, even_cnorm_g, even_cnorm_b, even_w_out, even_norm_ffn, even_ffn_w1,
              even_ffn_w3, even_ffn_w2, odd_norm_mix, odd_w_u, odd_b_u, odd_vnorm_g, odd_vnorm_b,
              odd_w_s, odd_b_s, odd_w_o, odd_norm_ffn, odd_router, odd_we_gate, odd_we_up,
              odd_we_down):
    for layer in range(DEPTH):
        i = layer // 2
        if layer % 2 == 0:
            x = x + _even_mixer(_rmsnorm(x, even_norm_mix[i]), even_w_in[i], even_q_norm[i],
                                even_k_norm[i], rel_bias, even_conv_w[i], even_conv_b[i],
                                even_cnorm_g[i], even_cnorm_b[i], even_w_out[i])
            x = x + _swiglu(_rmsnorm(x, even_norm_ffn[i]), even_ffn_w1[i], even_ffn_w3[i], even_ffn_w2[i])
        else:
            x = x + _gmlp_mixer(_rmsnorm(x, odd_norm_mix[i]), odd_w_u[i], odd_b_u[i], odd_vnorm_g[i],
                                odd_vnorm_b[i], odd_w_s[i], odd_b_s[i], odd_w_o[i])
            x = x + _moe_swiglu(_rmsnorm(x, odd_norm_ffn[i]), odd_router[i], odd_we_gate[i],
                                odd_we_up[i], odd_we_down[i])
    return x
```

```python
import numpy as np
from contextlib import ExitStack
import concourse.bass as bass
import concourse.mybir as mybir
from concourse.bass_utils import run_bass_kernel_spmd

F32 = mybir.dt.float32
BF16 = mybir.dt.bfloat16
AF = mybir.ActivationFunctionType
ALU = mybir.AluOpType

T = 2048
D = 2048
EPS = 1e-6
CAP = 640
CAPU = 640
NSL = CAP // 128


class Buf:
    __slots__ = ("name", "w", "r")

    def __init__(self, name=""):
        self.name = name
        self.w = None
        self.r = {}


def _merge(d, k, v):
    if d.get(k, 0) < v:
        d[k] = v


class Prog:
    ENGS = ("pe", "act", "dve", "pool", "sp")
    NDMA = 8

    def __init__(self, nc, stack):
        self.nc = nc
        self.q = {e: [] for e in self.ENGS}
        self.sems = {}
        self.cnt = {}
        for e in ("pe", "act", "dve", "pool"):
            self.sems[e] = stack.enter_context(nc.semaphore("s_" + e))
            self.cnt[e] = 0
        self.dma_i = {}
        for e in ("sp", "act", "pool"):
            for i in range(self.NDMA):
                k = "d_%s_%d" % (e, i)
                self.sems[k] = stack.enter_context(nc.semaphore(k))
                self.cnt[k] = 0
            self.dma_i[e] = 0
        self.waited = {e: {} for e in self.ENGS}
        self.nins = {e: 0 for e in self.ENGS}

    def _need(self, eng, deps):
        best = {}
        for d in deps:
            if d is None:
                continue
            k, v = d
            if eng == "pe" and k == "pe":
                continue
            _merge(best, k, v)
        for k, v in best.items():
            if self.waited[eng].get(k, 0) >= v:
                continue
            self.waited[eng][k] = v
            sem = self.sems[k]
            self.q[eng].append(lambda e, sem=sem, v=v: e.wait_ge(sem, v))

    @staticmethod
    def _deps(reads, writes):
        deps = []
        for b in reads:
            deps.append(b.w)
        for b in writes:
            deps.append(b.w)
            deps.extend(b.r.items())
        return deps

    @staticmethod
    def _commit(tok, reads, writes):
        k, v = tok
        for b in reads:
            _merge(b.r, k, v)
        for b in writes:
            b.w = tok
            b.r = {}

    def op(self, eng, fn, reads=(), writes=()):
        self._need(eng, self._deps(reads, writes))
        self.cnt[eng] += 1
        sem = self.sems[eng]
        self.q[eng].append(lambda e, fn=fn, sem=sem: fn(e).then_inc(sem, 1))
        self.nins[eng] += 1
        self._commit((eng, self.cnt[eng]), reads, writes)

    def dma(self, qeng, out, in_, reads=(), writes=(), **kw):
        deps = self._deps(reads, writes)
        i = self.dma_i[qeng]
        self.dma_i[qeng] += 1
        k = "d_%s_%d" % (qeng, i % self.NDMA)
        if self.cnt[k] > 0:
            deps.append((k, self.cnt[k]))
        self._need(qeng, deps)
        self.cnt[k] += 16
        sem = self.sems[k]
        self.q[qeng].append(
            lambda e, out=out, in_=in_, sem=sem, kw=kw: e.dma_start(out=out, in_=in_, **kw).then_inc(sem, 16))
        self.nins[qeng] += 1
        self._commit((k, self.cnt[k]), reads, writes)

    def wait_all(self, eng, bufs):
        deps = []
        for b in bufs:
            deps.append(b.w)
            deps.extend(b.r.items())
        self._need(eng, deps)

    def emit(self):
        nc = self.nc
        with nc.Block() as block:
            def mk(name):
                lst = self.q[name]

                def body(e):
                    for f in lst:
                        f(e)
                return body
            if self.q["sp"]:
                block.sync(mk("sp"))
            if self.q["act"]:
                block.scalar(mk("act"))
            if self.q["dve"]:
                block.vector(mk("dve"))
            if self.q["pool"]:
                block.gpsimd(mk("pool"))
            if self.q["pe"]:
                block.tensor(mk("pe"))


class Arena:
    def __init__(self, t, nbytes):
        self.t = t
        self.nbytes = nbytes
        self.top = 0
        self.live = []
        self.dead = []
        self.peak = 0

    def alloc(self, shape, dtype, nbufs=1, name=""):
        esz = 2 if dtype == BF16 else 4
        n = 1
        for s in shape:
            n *= s
        nb = n * esz
        assert nb % 4 == 0
        nb_al = (nb + 63) // 64 * 64
        start = self.top
        end = start + nb_al
        assert end <= self.nbytes, "arena overflow %s: %d > %d" % (name, end, self.nbytes)
        self.top = end
        self.peak = max(self.peak, end)
        ap = self.t[:, start // 4:(start + nb) // 4]
        if dtype != F32:
            ap = ap.bitcast(dtype)
        if len(shape) == 2:
            ap = ap.rearrange("p (a b) -> p a b", a=shape[0])
        elif len(shape) == 3:
            ap = ap.rearrange("p (a b c) -> p a b c", a=shape[0], b=shape[1])
        bufs = [Buf(name) for _ in range(nbufs)]
        keep = []
        for (s, e, toks) in self.dead:
            if s < end and e > start:
                for b in bufs:
                    for k, v in toks.items():
                        _merge(b.r, k, v)
                if s >= start and e <= end:
                    continue
            keep.append((s, e, toks))
        self.dead = keep
        self.live.append((start, end, bufs))
        return ap, (bufs[0] if nbufs == 1 else bufs)

    def mark(self):
        return (self.top, len(self.live))

    def release(self, mark):
        top, n = mark
        for (s, e, bufs) in self.live[n:]:
            toks = {}
            for b in bufs:
                if b.w is not None:
                    _merge(toks, b.w[0], b.w[1])
                for k, v in b.r.items():
                    _merge(toks, k, v)
            self.dead.append((s, e, toks))
        del self.live[n:]
        self.top = top


class Ring:
    def __init__(self, A, n, shape, dtype, name="", nbufs=1):
        self.slots = [A.alloc(shape, dtype, nbufs=nbufs, name="%s%d" % (name, i)) for i in range(n)]
        self.i = 0

    def next(self):
        s = self.slots[self.i % len(self.slots)]
        self.i += 1
        return s


class Ctx:
    pass


ARENA_BYTES = 206 * 1024
DILS = (1, 4, 16)
NBS = (16, 4, 1)
COFF = (0, 16, 32)

PP_CONVB, PP_CNG, PP_CNB, PP_BU, PP_QG, PP_KG, PP_N3, NPP = 0, 8, 16, 24, 40, 41, 42, 58
R_N0, R_N1, R_N2, R_N3, R_BU2, R_VNG, R_VNB, R_BS, R_ROUTER, NROWS = 0, 1, 2, 3, 4, 5, 6, 7, 8, 16


def din(C, name, shape, dtype=F32):
    if name not in C.din:
        C.din[name] = C.nc.dram_tensor(name, list(shape), dtype, kind="ExternalInput")
    return C.din[name]


def setup(nc, st):
    C = Ctx()
    C.nc = nc
    C.P = Prog(nc, st)
    C.din = {}
    C.arena_t = st.enter_context(nc.sbuf_tensor("arena", [128, ARENA_BYTES // 4], F32))
    C.A = Arena(C.arena_t, ARENA_BYTES)
    C.ps = st.enter_context(nc.psum_tensor("ps", [128, 8, 512], F32))
    C.bank = [Buf("bank%d" % i) for i in range(8)]
    C.rr = {}
    return C


def rr(C, key, items):
    i = C.rr.get(key, 0)
    C.rr[key] = i + 1
    return items[i % len(items)]


def consts(C):
    A, P, nc = C.A, C.P, C.nc
    C.identf, C.b_identf = A.alloc([128], F32, name="identf")
    C.identb, C.b_identb = A.alloc([128], BF16, name="identb")
    C.onesf, C.b_onesf = A.alloc([128], F32, name="onesf")
    C.onesb, C.b_onesb = A.alloc([128], BF16, name="onesb")
    C.antif, C.b_antif = A.alloc([128], F32, name="antif")
    C.triub, C.b_triub = A.alloc([128], BF16, name="triub")
    C.iorow, C.b_iorow = A.alloc([CAP], F32, name="iorow")
    C.iops, C.b_iops = A.alloc([NSL], F32, name="iops")
    C.pp, C.b_pp = A.alloc([NPP], F32, name="pp")
    C.cw, C.b_cw = A.alloc([8, 31], F32, name="cw")
    C.gvec, C.b_gvec = A.alloc([16], F32, name="gvec")
    P.op("pool", lambda e: e.memset(C.identf, 1.0), writes=[C.b_identf])
    P.op("pool", lambda e: e.affine_select(C.identf, C.identf, [[-1, 128]], ALU.is_equal, 0.0, base=0, channel_multiplier=1),
         reads=[C.b_identf], writes=[C.b_identf])
    P.op("pool", lambda e: e.memset(C.onesf, 1.0), writes=[C.b_onesf])
    P.op("pool", lambda e: e.memset(C.onesb, 1.0), writes=[C.b_onesb])
    P.op("pool", lambda e: e.memset(C.antif, 1.0), writes=[C.b_antif])
    P.op("pool", lambda e: e.affine_select(C.antif, C.antif, [[1, 128]], ALU.is_equal, 0.0, base=-127, channel_multiplier=1),
         reads=[C.b_antif], writes=[C.b_antif])
    P.op("pool", lambda e: e.memset(C.triub, 1.0), writes=[C.b_triub])
    P.op("pool", lambda e: e.affine_select(C.triub, C.triub, [[1, 128]], ALU.is_ge, 0.0, base=-1, channel_multiplier=-1),
         reads=[C.b_triub], writes=[C.b_triub])
    P.op("pool", lambda e: e.iota(C.iorow, [[1, CAP]], base=0, channel_multiplier=0, allow_small_or_imprecise_dtypes=True),
         writes=[C.b_iorow])
    P.op("pool", lambda e: e.iota(C.iops, [[128, NSL]], base=0, channel_multiplier=1, allow_small_or_imprecise_dtypes=True),
         writes=[C.b_iops])
    P.op("dve", lambda e: e.tensor_copy(C.identb, C.identf), reads=[C.b_identf], writes=[C.b_identb])
    P.dma("sp", C.pp, din(C, "pp", [128, NPP]).ap(), writes=[C.b_pp])
    P.dma("sp", C.cw, din(C, "cw", [128, 8, 31]).ap(), writes=[C.b_cw])
    sc = float(128 ** -0.5)
    P.op("dve", lambda e: e.tensor_scalar(C.gvec[:, 0:8], C.onesf[:, 0:8], C.pp[:, PP_QG:PP_QG + 1], sc, ALU.mult, ALU.mult),
         reads=[C.b_pp, C.b_onesf], writes=[C.b_gvec])
    P.op("dve", lambda e: e.tensor_scalar(C.gvec[:, 8:16], C.onesf[:, 0:8], C.pp[:, PP_KG:PP_KG + 1], None, ALU.mult),
         reads=[C.b_pp, C.b_onesf], writes=[C.b_gvec])


def row_bcast(C, r, n=2048, off=0):
    rows = din(C, "rows", [NROWS, 2048])
    return bass.AP(rows, r * 2048 + off, [[0, 128], [1, n]])


def wview(w_ap):
    return w_ap.rearrange("(kc p) f -> p kc f", p=128)


def load_slab(C, slot, wv, kc0, kc1, c0, w):
    ap, bufs = slot
    i = 0
    for k in range(kc0, kc1, 8):
        k2 = min(k + 8, kc1)
        C.P.dma("pool", ap[:, k - kc0:k2 - kc0, 0:w], wv[:, k:k2, c0:c0 + w], writes=[bufs[i]])
        i += 1
    return bufs[:i]


class SlabRing:
    def __init__(self, A, n, kcn, w, name="slab"):
        self.slots = [A.alloc([kcn, w], BF16, nbufs=(kcn + 7) // 8, name="%s%d" % (name, i)) for i in range(n)]
        self.slots = [(ap, b if isinstance(b, list) else [b]) for ap, b in self.slots]
        self.i = 0

    def next(self):
        s = self.slots[self.i % len(self.slots)]
        self.i += 1
        return s


def phase_norm(C, src_ap, src_bufs, row, tiles, hT, hTb, htok=None, htokb=None, post=None, tokout=None):
    A, P = C.A, C.P
    m = A.mark()
    gbc, gb = A.alloc([2048], F32, name="gbc")
    P.dma("sp", gbc, row_bcast(C, row), writes=[gb])
    xin = Ring(A, 2, [2048], F32, "xin")
    hbr = Ring(A, 2, [2048], BF16, "hb")
    junk, jb = A.alloc([2048], BF16, name="junk")
    nt = len(tiles)
    ss, ssb = A.alloc([nt], F32, nbufs=nt, name="ss")
    rs, rsb = A.alloc([nt], F32, nbufs=nt, name="rs")
    if nt == 1:
        ssb, rsb = [ssb], [rsb]
    for li, gi in enumerate(tiles):
        xt, xb = xin.next()
        P.dma("sp", xt, src_ap(gi), reads=src_bufs(gi), writes=[xb])
        P.op("act", lambda e, xt=xt, li=li: e.activation(junk, xt, AF.Square, accum_out=ss[:, li:li + 1]),
             reads=[xb], writes=[jb, ssb[li]])
        P.op("act", lambda e, li=li: e.activation(rs[:, li:li + 1], ss[:, li:li + 1], AF.Sqrt, scale=1.0 / D, bias=EPS),
             reads=[ssb[li]], writes=[rsb[li]])
        P.op("dve", lambda e, li=li: e.reciprocal(rs[:, li:li + 1], rs[:, li:li + 1]), reads=[rsb[li]], writes=[rsb[li]])
        if htok is None:
            hb, hbb = hbr.next()
        else:
            hb, hbb = htok[:, li, :], htokb[li]
        if post is not None:
            post(li, gi, xt, xb, rs[:, li:li + 1], rsb[li], gbc, gb)
        P.op("dve", lambda e, hb=hb, xt=xt, li=li: e.scalar_tensor_tensor(hb, xt, rs[:, li:li + 1], gbc, ALU.mult, ALU.mult),
             reads=[xb, rsb[li], gb], writes=[hbb])
        if tokout is not None:
            tokout(li, gi, hb, hbb)
        if hT is None:
            continue
        for half in range(2):
            bk = rr(C, "tbank", [4, 5, 6, 7])
            pb = C.ps[:, bk, :].bitcast(BF16)

            def tr(e, hb=hb, pb=pb, half=half):
                for k in range(8):
                    kc = half * 8 + k
                    ins = e.transpose(pb[:, k * 128:(k + 1) * 128], hb[:, kc * 128:(kc + 1) * 128], C.identb)
                return ins
            P.op("pe", tr, reads=[hbb, C.b_identb], writes=[C.bank[bk]])
            dst = hT[:, half * 8:(half + 1) * 8, li * 128:(li + 1) * 128]
            src = pb.rearrange("p (k t) -> p k t", k=8)
            if half == 0:
                P.op("act", lambda e, dst=dst, src=src: e.activation(dst, src, AF.Copy), reads=[C.bank[bk]], writes=[hTb[li]])
            else:
                P.op("dve", lambda e, dst=dst, src=src: e.tensor_copy(dst, src), reads=[C.bank[bk]], writes=[hTb[li]])
    A.release(m)


def mm_unitB(C, bk, slab, sbufs, col, kcn, rhs_fn, rbufs, ng=2):
    ps = C.ps

    def fn(e):
        for kc in range(kcn):
            for g in range(ng):
                ins = e.matmul(ps[:, bk + g, :], slab[:, kc, col:col + 128], rhs_fn(kc, g), start=(kc == 0), stop=(kc == kcn - 1))
        return ins
    C.P.op("pe", fn, reads=list(sbufs) + list(rbufs), writes=[C.bank[bk + g] for g in range(ng)])


def mm_unitA(C, bk, lhs_fn, lbufs, slab, sbufs, kcn, w, kc_off=0, start=True, stop=True):
    ps = C.ps

    def fn(e):
        for kc in range(kcn):
            ins = e.matmul(ps[:, bk, 0:w], lhs_fn(kc_off + kc), slab[:, kc, 0:w], start=(start and kc == 0), stop=(stop and kc == kcn - 1))
        return ins
    C.P.op("pe", fn, reads=list(sbufs) + list(lbufs), writes=[C.bank[bk]])


def phase_conv(C, h, hT, hTb, ob, obb, tails, tailsb):
    A, P, ps = C.A, C.P, C.ps
    m = A.mark()
    w_in = wview(din(C, "w_in", [2048, 5120]).ap())
    slabs = SlabRing(A, 4, 16, 256, "cslab")
    cc, ccb = A.alloc([8, 1024], F32, nbufs=8, name="cc")
    glur = Ring(A, 2, [1056], BF16, "glu")
    diagr = Ring(A, 2, [31, 128], BF16, "cdiag")
    idrep, idrepb = A.alloc([31, 128], BF16, name="idrep")
    sigr = Ring(A, 2, [1024], F32, "sig")
    sqf, sqfb = A.alloc([1024], F32, name="sqf")
    for k in range(31):
        P.op("dve", lambda e, k=k: e.tensor_copy(idrep[:, k, :], C.identb), reads=[C.b_identb], writes=[idrepb])
    rhs = lambda kc, g: hT[:, kc, g * 512:(g + 1) * 512]
    flat = lambda b0: ps[:, b0:b0 + 2, :].rearrange("p a b -> p (a b)")
    for cp in range(4):
        sv = slabs.next()
        bv = load_slab(C, sv, w_in, 0, 16, 3072 + cp * 256, 256)
        sg = slabs.next()
        bg = load_slab(C, sg, w_in, 0, 16, 4096 + cp * 256, 256)
        for c2 in range(2):
            c = cp * 2 + c2
            mm_unitB(C, 0, sv[0], bv, c2 * 128, 16, rhs, hTb)
            mm_unitB(C, 2, sg[0], bg, c2 * 128, 16, rhs, hTb)
            sig, sigb = sigr.next()
            P.op("act", lambda e, sig=sig: e.activation(sig, flat(2), AF.Sigmoid), reads=[C.bank[2], C.bank[3]], writes=[sigb])
            glu, glub = glur.next()
            if h == 0:
                P.op("dve", lambda e, glu=glu: e.memset(glu[:, 0:30], 0.0), writes=[glub])
            else:
                P.op("dve", lambda e, glu=glu, c=c: e.tensor_copy(glu[:, 0:30], tails[:, c, :]), reads=[tailsb], writes=[glub])
            P.op("dve", lambda e, glu=glu, sig=sig: e.tensor_tensor(glu[:, 30:1054], flat(0), sig, ALU.mult),
                 reads=[C.bank[0], C.bank[1], sigb], writes=[glub])
            if h == 0:
                P.op("dve", lambda e, glu=glu, c=c: e.tensor_copy(tails[:, c, :], glu[:, 1024:1054]), reads=[glub], writes=[tailsb])
            dg, dgb = diagr.next()
            a0 = C.cw[:, c, :]
            cwb = bass.AP(a0.tensor, a0.offset, [list(a0.ap[0]), [1, 31], [0, 128]])
            P.op("dve", lambda e, dg=dg, cwb=cwb: e.tensor_tensor(dg, idrep, cwb, ALU.mult), reads=[idrepb, C.b_cw], writes=[dgb])
            bk = rr(C, "cvpair", [4, 6])

            def conv(e, dg=dg, glu=glu, bk=bk):
                for g in range(2):
                    for k in range(31):
                        ins = e.matmul(ps[:, bk + g, :], dg[:, k, :], glu[:, k + g * 512:k + g * 512 + 512], start=(k == 0), stop=(k == 30))
                return ins
            P.op("pe", conv, reads=[dgb, glub], writes=[C.bank[bk], C.bank[bk + 1]])
            P.op("act", lambda e, c=c, bk=bk: e.activation(cc[:, c, :], flat(bk), AF.Identity, bias=C.pp[:, PP_CONVB + c:PP_CONVB + c + 1]),
                 reads=[C.bank[bk], C.bank[bk + 1], C.b_pp], writes=[ccb[c]])
    for c in range(8):
        accA = cc[:, c, :]

        def st1(e, accA=accA, c=c):
            for g in range(2):
                ins = e.matmul(ps[:, g, :], C.onesf, accA[:, g * 512:(g + 1) * 512], start=(c == 0), stop=(c == 7))
            return ins
        P.op("pe", st1, reads=[ccb[c], C.b_onesf], writes=[C.bank[0], C.bank[1]])
        P.op("act", lambda e, accA=accA: e.activation(sqf, accA, AF.Square), reads=[ccb[c]], writes=[sqfb])

        def st2(e, c=c):
            for g in range(2):
                ins = e.matmul(ps[:, 2 + g, :], C.onesf, sqf[:, g * 512:(g + 1) * 512], start=(c == 0), stop=(c == 7))
            return ins
        P.op("pe", st2, reads=[sqfb, C.b_onesf], writes=[C.bank[2], C.bank[3]])
    meant, meanb = A.alloc([1024], F32, name="mean")
    vart, varb = A.alloc([1024], F32, name="var")
    rstd, rstdb = A.alloc([1024], F32, name="rstd")
    yr = Ring(A, 2, [1024], F32, "y")
    P.op("act", lambda e: e.activation(meant, flat(0), AF.Copy, scale=1.0 / 1024), reads=[C.bank[0], C.bank[1]], writes=[meanb])
    P.op("dve", lambda e: e.tensor_tensor(vart, meant, meant, ALU.mult), reads=[meanb], writes=[varb])
    P.op("dve", lambda e: e.scalar_tensor_tensor(vart, flat(2), 1.0 / 1024, vart, ALU.mult, ALU.subtract),
         reads=[C.bank[2], C.bank[3]], writes=[varb])
    P.op("act", lambda e: e.activation(rstd, vart, AF.Sqrt, bias=EPS), reads=[varb], writes=[rstdb])
    P.op("dve", lambda e: e.reciprocal(rstd, rstd), reads=[rstdb], writes=[rstdb])
    for c in range(8):
        y, yb = yr.next()
        P.op("dve", lambda e, y=y, c=c: e.tensor_tensor(y, cc[:, c, :], meant, ALU.subtract), reads=[ccb[c], meanb], writes=[yb])
        P.op("dve", lambda e, y=y: e.tensor_tensor(y, y, rstd, ALU.mult), reads=[rstdb], writes=[yb])
        P.op("act", lambda e, y=y, c=c: e.activation(ob[:, c, h * 1024:(h + 1) * 1024], y, AF.Silu,
                                                      scale=C.pp[:, PP_CNG + c:PP_CNG + c + 1], bias=C.pp[:, PP_CNB + c:PP_CNB + c + 1]),
             reads=[yb, C.b_pp], writes=[obb[c]])
    A.release(m)


def phase_v(C, h, hT, hTb, vd, vdb):
    A, P, ps = C.A, C.P, C.ps
    m = A.mark()
    w_in = wview(din(C, "w_in", [2048, 5120]).ap())
    slabs = SlabRing(A, 3, 16, 256, "vslab")
    vtok, vtokb = A.alloc([8, 1024], BF16, nbufs=8, name="vtok")
    for s4 in range(4):
        sl = slabs.next()
        sb = load_slab(C, sl, w_in, 0, 16, 2048 + s4 * 256, 256)
        for li in range(8):
            bk = rr(C, "vbank", [0, 1, 2, 3])
            mm_unitA(C, bk, lambda kc, li=li: hT[:, kc, li * 128:(li + 1) * 128], [hTb[li]], sl[0], sb, 16, 256)
            dst = vtok[:, li, s4 * 256:(s4 + 1) * 256]
            if li % 2 == 0:
                P.op("act", lambda e, dst=dst, bk=bk: e.activation(dst, ps[:, bk, 0:256], AF.Copy), reads=[C.bank[bk]], writes=[vtokb[li]])
            else:
                P.op("dve", lambda e, dst=dst, bk=bk: e.tensor_copy(dst, ps[:, bk, 0:256]), reads=[C.bank[bk]], writes=[vtokb[li]])
    for li in range(8):
        gi = h * 8 + li
        P.dma("sp", vd.ap()[gi * 128:(gi + 1) * 128, :], vtok[:, li, :], reads=[vtokb[li]], writes=[vdb[gi]])
    A.release(m)


def phase_qk(C, h, hT, hTb, qkT, qkb):
    A, P, ps = C.A, C.P, C.ps
    m = A.mark()
    w_in = wview(din(C, "w_in", [2048, 5120]).ap())
    slabs = SlabRing(A, 3, 16, 256, "qslab")
    rawr = Ring(A, 2, [1024], F32, "raw")
    sqr = Ring(A, 2, [1024], BF16, "sq")
    rtr = Ring(A, 2, [1024], F32, "rt")
    rhs = lambda kc, g: hT[:, kc, g * 512:(g + 1) * 512]
    flat = lambda b0: ps[:, b0:b0 + 2, :].rearrange("p a b -> p (a b)")
    for s in range(8):
        sl = slabs.next()
        sb = load_slab(C, sl, w_in, 0, 16, s * 256, 256)
        for jj in range(2):
            j = s * 2 + jj
            bk = rr(C, "qkpair", [0, 2, 4])
            mm_unitB(C, bk, sl[0], sb, jj * 128, 16, rhs, hTb)
            raw, rawb = rawr.next()
            sq, sqb = sqr.next()
            rt, rtb = rtr.next()
            P.op("act", lambda e, raw=raw, bk=bk: e.activation(raw, flat(bk), AF.Copy), reads=[C.bank[bk], C.bank[bk + 1]], writes=[rawb])
            P.op("act", lambda e, sq=sq, bk=bk: e.activation(sq, flat(bk), AF.Square), reads=[C.bank[bk], C.bank[bk + 1]], writes=[sqb])

            def ssq(e, sq=sq):
                for g in range(2):
                    ins = e.matmul(ps[:, 6 + g, :], C.onesb, sq[:, g * 512:(g + 1) * 512], start=True, stop=True)
                return ins
            P.op("pe", ssq, reads=[sqb, C.b_onesb], writes=[C.bank[6], C.bank[7]])
            P.op("act", lambda e, rt=rt: e.activation(rt, flat(6), AF.Sqrt, scale=1.0 / 128, bias=EPS), reads=[C.bank[6], C.bank[7]], writes=[rtb])
            P.op("dve", lambda e, rt=rt: e.reciprocal(rt, rt), reads=[rtb], writes=[rtb])
            P.op("dve", lambda e, raw=raw, rt=rt, j=j: e.scalar_tensor_tensor(qkT[:, j, h * 1024:(h + 1) * 1024], raw, C.gvec[:, j:j + 1], rt, ALU.mult, ALU.mult),
                 reads=[rawb, rtb, C.b_gvec], writes=[qkb[j]])
    A.release(m)


def phase_bias(C, biasT, biasb):
    A, P, ps, nc = C.A, C.P, C.ps, C.nc
    m = A.mark()
    rb, rbb = A.alloc([8], F32, name="rbaug")
    oh, ohb = A.alloc([1152], F32, name="ohu")
    usb, usbb = A.alloc([1152], F32, name="usb")
    hs, hsb = A.alloc([8, 256], F32, name="hs")
    ud = nc.dram_tensor("ud", [8, 1152], F32)
    udb = Buf("ud")
    P.op("dve", lambda e: e.memset(rb[0:64, :], -30000.0), writes=[rbb])
    P.dma("sp", rb[0:32, :], din(C, "rel_bias", [32, 8]).ap(), writes=[rbb])
    P.dma("sp", oh[0:64, :], din(C, "ohu", [64, 1152]).ap(), writes=[ohb])
    for b in range(3):
        P.op("pe", lambda e, b=b: e.matmul(ps[0:8, b, 0:384], rb[0:64, :], oh[0:64, b * 384:(b + 1) * 384], start=True, stop=True),
             reads=[rbb, ohb], writes=[C.bank[b]])
        P.op("act", lambda e, b=b: e.activation(usb[0:8, b * 384:(b + 1) * 384], ps[0:8, b, 0:384], AF.Copy), reads=[C.bank[b]], writes=[usbb])
    P.dma("sp", ud.ap(), usb[0:8, :], reads=[usbb], writes=[udb])
    for b in range(3):
        src = bass.AP(ud, b * 384, [[1, 128], [1152, 8], [1, 256]])
        P.dma("sp", hs, src, reads=[udb], writes=[hsb])
        for hd in range(8):
            bk = rr(C, "bbank", [3, 4, 5, 6])
            P.op("pe", lambda e, hd=hd, bk=bk: e.matmul(ps[:, bk, 0:256], C.antif, hs[:, hd, :], start=True, stop=True),
                 reads=[hsb, C.b_antif], writes=[C.bank[bk]])
            P.op("act", lambda e, b=b, hd=hd, bk=bk: e.activation(biasT[:, b * 8 + hd, :], ps[:, bk, 0:256], AF.Copy),
                 reads=[C.bank[bk]], writes=[biasb])
    A.release(m)


def phase_attn(C, qkT, qkb, vd, vdb, biasT, biasb):
    A, P, ps = C.A, C.P, C.ps
    m = A.mark()
    vhr = Ring(A, 2, [48, 128], BF16, "vh", nbufs=21)
    od, odb = A.alloc([2, 2048], F32, name="od")
    ptr = Ring(A, 8, [256], BF16, "pt")
    LOOK = 2
    for hd in range(8):
        vh, vhbl = vhr.next()
        vhbm = {}
        ib = 0
        for b in range(3):
            dil, nb = DILS[b], NBS[b]
            for r in range(dil):
                src = bass.AP(vd, r * 1024 + hd * 128, [[dil * 1024, 128], [128 * dil * 1024, nb], [1, 128]])
                c0 = COFF[b] + r * nb
                P.dma("sp", vh[:, c0:c0 + nb, :], src, reads=vdb, writes=[vhbl[ib]])
                vhbm[(b, r)] = vhbl[ib]
                ib += 1
        combos = [(b, r, n) for b in range(3) for r in range(DILS[b]) for n in range(NBS[b])]
        pts = {}

        def stage_a(i, hd=hd):
            b, r, n = combos[i]
            dil, nb = DILS[b], NBS[b]
            start = n * 128 * dil + r
            nq = 256 if n < nb - 1 else 128
            kT = qkT[:, 8 + hd, start:start + 127 * dil + 1:dil]
            qT = qkT[:, hd, start:start + (nq - 1) * dil + 1:dil]
            bk = rr(C, "sbank", [0, 1, 2, 3])

            def sc(e, bk=bk, kT=kT, qT=qT, nq=nq, b=b):
                e.matmul(ps[:, bk, 0:nq], kT, qT, start=True, stop=False)
                return e.matmul(ps[:, bk, 0:nq], C.identb, biasT[:, b * 8 + hd, 0:nq], start=False, stop=True)
            P.op("pe", sc, reads=[qkb[hd], qkb[8 + hd], biasb, C.b_identb], writes=[C.bank[bk]])
            p_t, p_b = ptr.next()
            P.op("act", lambda e, bk=bk, p_t=p_t, nq=nq: e.activation(p_t[:, 0:nq], ps[:, bk, 0:nq], AF.Exp), reads=[C.bank[bk]], writes=[p_b])
            pts[i] = (p_t, p_b)

        def stage_b(i, hd=hd, vh=vh):
            b, r, n = combos[i]
            dil, nb = DILS[b], NBS[b]
            start = n * 128 * dil + r
            p_t, p_b = pts[i]
            prev = pts[i - 1] if n > 0 else None
            c = COFF[b] + r * nb + n
            ob_ = rr(C, "obank", [4, 5, 6, 7])

            def pv(e, ob_=ob_, prev=prev, p_t=p_t, c=c, vh=vh):
                for grp in range(2):
                    lhs_prev = (vh[:, c - 1, :] if grp == 0 else C.onesb) if prev is not None else None
                    lhs_cur = vh[:, c, :] if grp == 0 else C.onesb
                    o = ps[:, ob_, grp * 128:(grp + 1) * 128]
                    if prev is not None:
                        e.matmul(o, lhs_prev, prev[0][:, 128:256], start=True, stop=False)
                    ins = e.matmul(o, lhs_cur, p_t[:, 0:128], start=(prev is None), stop=True)
                return ins
            rd = [vhbm[(b, r)], p_b, C.b_onesb] + ([prev[1]] if prev is not None else [])
            P.op("pe", pv, reads=rd, writes=[C.bank[ob_]])
            odv = od[:, :, start:start + 127 * dil + 1:dil]
            psv = ps[:, ob_, 0:256].rearrange("p (a t) -> p a t", a=2)
            if b == 0:
                P.op("act", lambda e, odv=odv, psv=psv: e.activation(odv, psv, AF.Copy), reads=[C.bank[ob_]], writes=[odb])
            else:
                P.op("dve", lambda e, odv=odv, psv=psv: e.tensor_tensor(odv, odv, psv, ALU.add), reads=[C.bank[ob_]], writes=[odb])
            if i - 1 in pts and (n == 0 or True):
                pass
        N = len(combos)
        for i in range(N + LOOK):
            if i < N:
                stage_a(i)
            if i - LOOK >= 0:
                stage_b(i - LOOK)
        P.op("dve", lambda e: e.reciprocal(od[:, 1, :], od[:, 1, :]), reads=[odb], writes=[odb])
        P.op("dve", lambda e, hd=hd: e.tensor_tensor(qkT[:, hd, :], od[:, 0, :], od[:, 1, :], ALU.mult), reads=[odb], writes=[qkb[hd]])
    A.release(m)


def phase_outproj(C, wname, wshape, kcn, lhs_fn, lbufs_fn, tiles, res_ap, res_bufs, dst_ap, dst_bufs, kslab=None):
    A, P, ps = C.A, C.P, C.ps
    m = A.mark()
    wv = wview(din(C, wname, wshape).ap())
    kslab = kslab or kcn
    nks = kcn // kslab
    slabs = SlabRing(A, 3 if kslab <= 16 else 2, kslab, 512, "oslab")
    resr = Ring(A, 5, [512], F32, "res")
    nt = len(tiles)
    for cg in range(4):
        if nks == 1:
            sl = slabs.next()
            sb = load_slab(C, sl, wv, 0, kcn, cg * 512, 512)
            for li, gi in enumerate(tiles):
                bk = rr(C, "opbank", [0, 1, 2, 3, 4, 5, 6, 7])
                mm_unitA(C, bk, lambda kc, li=li: lhs_fn(kc, li), lbufs_fn(li), sl[0], sb, kcn, 512)
                _op_evac(C, resr, bk, gi, cg, res_ap, res_bufs, dst_ap, dst_bufs)
        else:
            assert nt <= 8
            for ks in range(nks):
                sl = slabs.next()
                sb = load_slab(C, sl, wv, ks * kslab, (ks + 1) * kslab, cg * 512, 512)
                for li, gi in enumerate(tiles):
                    mm_unitA(C, li, lambda kc, li=li: lhs_fn(kc, li), lbufs_fn(li), sl[0], sb, kslab, 512,
                             kc_off=ks * kslab, start=(ks == 0), stop=(ks == nks - 1))
                    if ks == nks - 1:
                        _op_evac(C, resr, li, gi, cg, res_ap, res_bufs, dst_ap, dst_bufs)
    A.release(m)


def _op_evac(C, resr, bk, gi, cg, res_ap, res_bufs, dst_ap, dst_bufs):
    P, ps = C.P, C.ps
    rt, rtb = resr.next()
    P.dma("act", rt, res_ap(gi, cg), reads=res_bufs(gi, cg), writes=[rtb])
    P.op("dve", lambda e, rt=rt, bk=bk: e.tensor_tensor(rt, rt, ps[:, bk, :], ALU.add), reads=[C.bank[bk]], writes=[rtb])
    P.dma("sp", dst_ap(gi, cg), rt, reads=[rtb], writes=dst_bufs(gi, cg))


GELU_C = 0.044715
GELU_K = 1.5957691216057308


def flat2(C, bk, n=512):
    if n == 512:
        return C.ps[:, bk:bk + 2, :].rearrange("p a b -> p (a b)")
    return C.ps[:, bk:bk + 2, 0:n]


def out_tile_ap(out, gi, c0, w):
    return out.ap()[gi * 128:(gi + 1) * 128, c0:c0 + w]


def layer0_ffn(C, out, outb):
    A, P, ps = C.A, C.P, C.ps
    w1 = wview(din(C, "w1", [2048, 5632]).ap())
    w3 = wview(din(C, "w3", [2048, 5632]).ap())
    for h in range(2):
        tiles = list(range(h * 8, h * 8 + 8))
        m = A.mark()
        hid, hidb = A.alloc([44, 1024], BF16, nbufs=44, name="hid")
        m2 = A.mark()
        hT, hTb = A.alloc([16, 1024], BF16, nbufs=8, name="hT")
        phase_norm(C, lambda gi: out.ap()[gi * 128:(gi + 1) * 128, :], lambda gi: outb[gi], R_N1, tiles, hT, hTb)
        slabs = SlabRing(A, 4, 16, 256, "fslab")
        sgr = Ring(A, 2, [1024], F32, "sg")
        rhs = lambda kc, g, hT=hT: hT[:, kc, g * 512:(g + 1) * 512]
        for s in range(22):
            s1 = slabs.next()
            b1 = load_slab(C, s1, w1, 0, 16, s * 256, 256)
            s3 = slabs.next()
            b3 = load_slab(C, s3, w3, 0, 16, s * 256, 256)
            for jj in range(2):
                fb = s * 2 + jj
                bk1 = rr(C, "ffpair", [0, 2, 4, 6])
                bk3 = rr(C, "ffpair", [0, 2, 4, 6])
                mm_unitB(C, bk1, s1[0], b1, jj * 128, 16, rhs, hTb)
                mm_unitB(C, bk3, s3[0], b3, jj * 128, 16, rhs, hTb)
                sg, sgb = sgr.next()
                P.op("act", lambda e, sg=sg, bk1=bk1: e.activation(sg, flat2(C, bk1), AF.Silu),
                     reads=[C.bank[bk1], C.bank[bk1 + 1]], writes=[sgb])
                P.op("dve", lambda e, sg=sg, bk3=bk3, fb=fb: e.tensor_tensor(hid[:, fb, :], sg, flat2(C, bk3), ALU.mult),
                     reads=[sgb, C.bank[bk3], C.bank[bk3 + 1]], writes=[hidb[fb]])
        A.release(m2)
        phase_outproj(C, "w2", [5632, 2048], 44, lambda kc, li, hid=hid: hid[:, kc, li * 128:(li + 1) * 128], lambda li, hidb=hidb: hidb, tiles,
                      lambda gi, cg: out_tile_ap(out, gi, cg * 512, 512), lambda gi, cg: [outb[gi][cg]],
                      lambda gi, cg: out_tile_ap(out, gi, cg * 512, 512), lambda gi, cg: [outb[gi][cg]], kslab=22)
        A.release(m)


def gelu_chain(C, xt, xb, t1, t1b, sgm, sgmb, shape_free):
    P = C.P
    P.op("act", lambda e: e.activation(t1, xt, AF.Square), reads=[xb], writes=[t1b])
    P.op("dve", lambda e: e.tensor_scalar(t1, t1, GELU_C, 1.0, ALU.mult, ALU.add), reads=[t1b], writes=[t1b])
    P.op("dve", lambda e: e.tensor_tensor(t1, t1, xt, ALU.mult), reads=[t1b, xb], writes=[t1b])
    P.op("act", lambda e: e.activation(sgm, t1, AF.Sigmoid, scale=GELU_K), reads=[t1b], writes=[sgmb])


def layer1_mixer(C, out, outb):
    A, P, ps, nc = C.A, C.P, C.ps, C.nc
    C.accd = nc.dram_tensor("accd", [2048 + CAP, 2048], F32)
    C.accb = [[Buf("acc%d_%d" % (i, j)) for j in range(4)] for i in range(16)]
    w_u = wview(din(C, "w_u", [2048, 4096]).ap())
    m0 = A.mark()
    wsTb, wsTbb = A.alloc([16, 128], BF16, name="wsTb")
    bsbc, bsbcb = A.alloc([16, 128], F32, name="bsbc")
    mm_ = A.mark()
    wsf, wsfb = A.alloc([16, 128], F32, name="wsf")
    P.dma("sp", wsf, din(C, "wsT", [128, 16, 128]).ap(), writes=[wsfb])
    P.op("pool", lambda e: e.affine_select(wsf, wsf, [[0, 16], [1, 128]], ALU.is_ge, 0.0, base=0, channel_multiplier=-1),
         reads=[wsfb], writes=[wsfb])
    P.op("dve", lambda e: e.tensor_copy(wsTb, wsf), reads=[wsfb], writes=[wsTbb])
    A.release(mm_)
    P.dma("sp", bsbc.rearrange("p a b -> p (a b)"), row_bcast(C, R_BS), writes=[bsbcb])
    for h in range(2):
        tiles = list(range(h * 8, h * 8 + 8))
        m = A.mark()
        gT, gTb = A.alloc([16, 1024], BF16, nbufs=16, name="gT")
        vln, vlnb = A.alloc([8, 2048], BF16, nbufs=8, name="vln")
        m2 = A.mark()
        hT, hTb = A.alloc([16, 1024], BF16, nbufs=8, name="hT")
        phase_norm(C, lambda gi: out.ap()[gi * 128:(gi + 1) * 128, :], lambda gi: outb[gi], R_N2, tiles, hT, hTb)
        m3 = A.mark()
        bubc, bubcb = A.alloc([2048], F32, name="bubc")
        P.dma("sp", bubc, row_bcast(C, R_BU2), writes=[bubcb])
        s1, s1b = A.alloc([8, 4], F32, nbufs=8, name="s1")
        s2, s2b = A.alloc([8, 4], F32, nbufs=8, name="s2")
        m4 = A.mark()
        slabs = SlabRing(A, 2, 16, 512, "vslab")
        ztr = Ring(A, 4, [512], F32, "zt")
        t1r = Ring(A, 4, [512], F32, "t1")
        sgr = Ring(A, 4, [512], F32, "sgm")
        vtr = Ring(A, 4, [512], F32, "vt")
        junk, junkb = A.alloc([512], F32, name="junk")
        for cg in range(4):
            sl = slabs.next()
            sb = load_slab(C, sl, w_u, 0, 16, 2048 + cg * 512, 512)
            for li in range(8):
                bk = rr(C, "gvbank", [0, 1, 2, 3, 4, 5])
                mm_unitA(C, bk, lambda kc, li=li, hT=hT: hT[:, kc, li * 128:(li + 1) * 128], [hTb[li]], sl[0], sb, 16, 512)
                zt, ztb = ztr.next()
                t1, t1b = t1r.next()
                sgm, sgmb = sgr.next()
                vt, vtb = vtr.next()
                P.op("dve", lambda e, zt=zt, bk=bk, cg=cg: e.tensor_tensor(zt, ps[:, bk, :], bubc[:, cg * 512:(cg + 1) * 512], ALU.add),
                     reads=[C.bank[bk], bubcb], writes=[ztb])
                gelu_chain(C, zt, ztb, t1, t1b, sgm, sgmb, None)
                P.op("dve", lambda e, zt=zt, sgm=sgm, vt=vt, li=li, cg=cg: e.scalar_tensor_tensor(vt, zt, 1.0, sgm, ALU.mult, ALU.mult, accum_out=s1[:, li, cg:cg + 1]),
                     reads=[ztb, sgmb], writes=[vtb, s1b[li]])
                P.op("act", lambda e, vt=vt, li=li, cg=cg: e.activation(junk, vt, AF.Square, accum_out=s2[:, li, cg:cg + 1]),
                     reads=[vtb], writes=[junkb, s2b[li]])
                P.op("act", lambda e, vt=vt, li=li, cg=cg: e.activation(vln[:, li, cg * 512:(cg + 1) * 512], vt, AF.Copy),
                     reads=[vtb], writes=[vlnb[li]])
        A.release(m4)
        vgbc, vgbcb = A.alloc([2048], F32, name="vgbc")
        vbbc, vbbcb = A.alloc([2048], F32, name="vbbc")
        P.dma("sp", vgbc, row_bcast(C, R_VNG), writes=[vgbcb])
        P.dma("sp", vbbc, row_bcast(C, R_VNB), writes=[vbbcb])
        st_, stb = A.alloc([8, 4], F32, nbufs=8, name="lnst")
        lnr = Ring(A, 2, [2048], F32, "lnt")
        for li in range(8):
            mean, var, rstd, sm2 = (st_[:, li, k:k + 1] for k in range(4))
            P.op("dve", lambda e, li=li, mean=mean: e.tensor_reduce(mean, s1[:, li, :], mybir.AxisListType.X, ALU.add), reads=[s1b[li]], writes=[stb[li]])
            P.op("dve", lambda e, li=li, sm2=sm2: e.tensor_reduce(sm2, s2[:, li, :], mybir.AxisListType.X, ALU.add), reads=[s2b[li]], writes=[stb[li]])
            P.op("dve", lambda e, mean=mean: e.tensor_scalar(mean, mean, 1.0 / 2048, None, ALU.mult), reads=[stb[li]], writes=[stb[li]])
            P.op("dve", lambda e, mean=mean, var=var: e.tensor_tensor(var, mean, mean, ALU.mult), reads=[stb[li]], writes=[stb[li]])
            P.op("dve", lambda e, var=var, sm2=sm2: e.scalar_tensor_tensor(var, sm2, 1.0 / 2048, var, ALU.mult, ALU.subtract), reads=[stb[li]], writes=[stb[li]])
            P.op("act", lambda e, var=var, rstd=rstd: e.activation(rstd, var, AF.Sqrt, bias=EPS), reads=[stb[li]], writes=[stb[li]])
            P.op("dve", lambda e, rstd=rstd: e.reciprocal(rstd, rstd), reads=[stb[li]], writes=[stb[li]])
            lt, ltb = lnr.next()
            P.op("dve", lambda e, lt=lt, li=li, mean=mean, rstd=rstd: e.tensor_scalar(lt, vln[:, li, :], mean, rstd, ALU.subtract, ALU.mult),
                 reads=[vlnb[li], stb[li]], writes=[ltb])
            P.op("dve", lambda e, lt=lt: e.tensor_tensor(lt, lt, vgbc, ALU.mult), reads=[vgbcb], writes=[ltb])
            P.op("dve", lambda e, lt=lt, li=li: e.tensor_tensor(vln[:, li, :], lt, vbbc, ALU.add), reads=[ltb, vbbcb], writes=[vlnb[li]])
        A.release(m3)
        slabs = SlabRing(A, 3, 16, 256, "uslab")
        xr = Ring(A, 3, [1024], F32, "xu")
        t1r = Ring(A, 3, [1024], F32, "t1u")
        sgr = Ring(A, 3, [1024], F32, "sgu")
        tmr = Ring(A, 3, [1024], F32, "tmu")
        rhs = lambda kc, g, hT=hT: hT[:, kc, g * 512:(g + 1) * 512]
        for s in range(8):
            sl = slabs.next()
            sb = load_slab(C, sl, w_u, 0, 16, s * 256, 256)
            for jj in range(2):
                g = s * 2 + jj
                bk = rr(C, "gupair", [0, 2])
                mm_unitB(C, bk, sl[0], sb, jj * 128, 16, rhs, hTb)
                xt, xb = xr.next()
                t1, t1b = t1r.next()
                sgm, sgmb = sgr.next()
                tm, tmb = tmr.next()
                P.op("act", lambda e, xt=xt, bk=bk, g=g: e.activation(xt, flat2(C, bk), AF.Identity, bias=C.pp[:, PP_BU + g:PP_BU + g + 1]),
                     reads=[C.bank[bk], C.bank[bk + 1], C.b_pp], writes=[xb])
                gelu_chain(C, xt, xb, t1, t1b, sgm, sgmb, None)
                P.op("dve", lambda e, xt=xt, sgm=sgm: e.tensor_tensor(xt, xt, sgm, ALU.mult), reads=[sgmb], writes=[xb])
                gb_ = rr(C, "ggpair", [4, 6])

                def gate(e, g=g, gb_=gb_, vln=vln):
                    for li in range(8):
                        ins = e.matmul(ps[:, gb_ + li // 4, (li % 4) * 128:(li % 4 + 1) * 128], vln[:, li, g * 128:(g + 1) * 128], wsTb[:, g, :],
                                       start=True, stop=True)
                    return ins
                P.op("pe", gate, reads=vlnb + [wsTbb], writes=[C.bank[gb_], C.bank[gb_ + 1]])
                a0 = bsbc[:, g, :]
                zap = bass.AP(a0.tensor, a0.offset, [list(a0.ap[0]), [0, 8], [1, 128]])
                P.op("dve", lambda e, tm=tm, gb_=gb_, zap=zap: e.tensor_tensor(tm.rearrange("p (a b) -> p a b", a=8), flat2(C, gb_).rearrange("p (a b) -> p a b", a=8), zap, ALU.add),
                     reads=[C.bank[gb_], C.bank[gb_ + 1], bsbcb], writes=[tmb])
                P.op("dve", lambda e, tm=tm, xt=xt, g=g: e.tensor_tensor(gT[:, g, :], tm, xt, ALU.mult), reads=[tmb, xb], writes=[gTb[g]])
        A.release(m2)
        w_o_name = "w_o"
        phase_outproj(C, w_o_name, [2048, 2048], 16, lambda kc, li, gT=gT: gT[:, kc, li * 128:(li + 1) * 128], lambda li, gTb=gTb: gTb, tiles,
                      lambda gi, cg: out_tile_ap(out, gi, cg * 512, 512), lambda gi, cg: [outb[gi][cg]],
                      lambda gi, cg: out_tile_ap(C.accd, gi, cg * 512, 512), lambda gi, cg: [C.accb[gi][cg]])
        A.release(m)
    A.release(m0)


def zbc(ap3, n):
    return bass.AP(ap3.tensor, ap3.offset, [list(ap3.ap[0]), list(ap3.ap[1]), [0, n]])


def load_slab_c(C, slot, src2d, kcn, w):
    ap, bufs = slot
    i = 0
    for k in range(0, kcn, 8):
        k2 = min(k + 8, kcn)
        C.P.dma("pool", ap[:, k:k2, 0:w].rearrange("p a b -> p (a b)"), src2d[:, k * w:k2 * w], writes=[bufs[i]])
        i += 1
    return bufs[:i]


class WStream:
    def __init__(self, C, ring, loads):
        self.C, self.ring, self.loads = C, ring, loads
        self.n = len(ring.slots)
        self.issued = []
        self.next_use = 0
        for _ in range(self.n):
            self._issue()

    def _issue(self):
        i = len(self.issued)
        if i >= len(self.loads):
            return
        slot = self.ring.slots[i % self.n]
        bufs = load_slab_c(self.C, slot, *self.loads[i])
        self.issued.append((slot[0], bufs))

    def get(self):
        r = self.issued[self.next_use]
        self.next_use += 1
        return r

    def done(self):
        self._issue()


def layer1_moe(C, out, outb):
    A, P, ps, nc = C.A, C.P, C.ps, C.nc
    H = CAPU // 2
    m0 = A.mark()
    lg, lgb = A.alloc([16, 8], F32, nbufs=16, name="lg")
    top8, top8b = A.alloc([16, 8], F32, name="top8")
    Mk, Mkb_ = A.alloc([16, 8], F32, name="Mk")
    Gt, Gtb = A.alloc([16, 8], F32, name="Gt")
    excl, exclb = A.alloc([16, 8], F32, name="excl")
    exclm, exclmb = A.alloc([16, 8], F32, name="exclm")
    Mkh, Mkhb = A.alloc([16, 8], BF16, name="Mkh")
    gsc, gscb = A.alloc([16, 4], F32, name="gsc")
    I32 = mybir.dt.int32
    accd, accb = C.accd, C.accb
    accall = Buf("accall")
    gsl, gslb = A.alloc([8, NSL], F32, name="gsl")
    idxs, idxsb = A.alloc([8, 8], I32, name="idxs")
    hted = nc.dram_tensor("hted", [8, 128, 16 * CAPU], BF16)
    htedb = [Buf("hted%d" % e) for e in range(8)]
    m1 = A.mark()
    htokd = nc.dram_tensor("htokd", [2048, 2048], BF16)
    htokdb = [Buf("htokd%d" % i) for i in range(16)]
    m1b = A.mark()
    gw, gwb = A.alloc([16, 8], F32, name="gw")
    P.dma("sp", gw, din(C, "wrfm", [128, 16, 8]).ap(), writes=[gwb])
    a0 = C.pp[:, PP_N3:PP_N3 + 16]
    gb3 = bass.AP(a0.tensor, a0.offset, [list(a0.ap[0]), [1, 16], [0, 8]])
    P.op("dve", lambda e: e.tensor_tensor(gw, gw, gb3, ALU.mult), reads=[C.b_pp], writes=[gwb])
    xTr = Ring(A, 2, [16, 128], F32, "xT", nbufs=4)

    def post(li, gi, xt, xb, rs, rsb, gbc, gb):
        xTt, xTb = xTr.next()
        for j in range(4):
            bk = rr(C, "rtbank", [0, 1, 2, 3, 4, 5])

            def tr(e, j=j, bk=bk, xt=xt):
                for k in range(4):
                    kc = j * 4 + k
                    ins = e.transpose(ps[:, bk, k * 128:(k + 1) * 128], xt[:, kc * 128:(kc + 1) * 128], C.identf)
                return ins
            P.op("pe", tr, reads=[xb, C.b_identf], writes=[C.bank[bk]])
            dst = xTt[:, j * 4:(j + 1) * 4, :]
            src = ps[:, bk, :].rearrange("p (k t) -> p k t", k=4)
            if j % 2 == 0:
                P.op("act", lambda e, dst=dst, src=src: e.activation(dst, src, AF.Copy), reads=[C.bank[bk]], writes=[xTb[j]])
            else:
                P.op("dve", lambda e, dst=dst, src=src: e.tensor_copy(dst, src), reads=[C.bank[bk]], writes=[xTb[j]])
        lb_ = rr(C, "lgbank", [6, 7])

        def rt(e, xTt=xTt, lb_=lb_):
            for kc in range(16):
                ins = e.matmul(ps[:, lb_, 0:8], xTt[:, kc, :], gw[:, kc, :], start=(kc == 0), stop=(kc == 15))
            return ins
        P.op("pe", rt, reads=xTb + [gwb], writes=[C.bank[lb_]])
        P.op("act", lambda e, li=li, lb_=lb_, rs=rs: e.activation(lg[:, li, :], ps[:, lb_, 0:8], AF.Copy, scale=rs), reads=[C.bank[lb_], rsb], writes=[lgb[li]])
    def tokout(li, gi, hb, hbb):
        P.dma("sp", htokd.ap()[gi * 128:(gi + 1) * 128, :], hb, reads=[hbb], writes=[htokdb[gi]])
    phase_norm(C, lambda gi: accd.ap()[gi * 128:(gi + 1) * 128, :], lambda gi: accb[gi], R_N3, list(range(16)), None, None,
               post=post, tokout=tokout)
    A.release(m1b)
    for li in range(16):
        P.op("dve", lambda e, li=li: e.max(top8[:, li, :], lg[:, li, :]), reads=[lgb[li]], writes=[top8b])
    m1v = zbc(top8[:, :, 0:1], 8)
    m2v = zbc(top8[:, :, 1:2], 8)
    mtmp, mtmpb = A.alloc([16, 8], F32, name="mtmp")
    P.op("dve", lambda e: e.tensor_tensor(gsc[:, :, 2:3], top8[:, :, 0:1], top8[:, :, 1:2], ALU.subtract), reads=[top8b], writes=[gscb])
    P.op("act", lambda e: e.activation(gsc[:, :, 0:1], gsc[:, :, 2:3], AF.Sigmoid), reads=[gscb], writes=[gscb])
    P.op("dve", lambda e: e.tensor_scalar(gsc[:, :, 1:2], gsc[:, :, 0:1], -1.0, 1.0, ALU.mult, ALU.add), reads=[gscb], writes=[gscb])
    P.op("dve", lambda e: e.tensor_tensor(Mk, lg, m1v, ALU.is_equal), reads=lgb + [top8b], writes=[Mkb_])
    P.op("dve", lambda e: e.tensor_tensor(mtmp, lg, m2v, ALU.is_equal), reads=lgb + [top8b], writes=[mtmpb])
    P.op("dve", lambda e: e.tensor_tensor(Gt, Mk, zbc(gsc[:, :, 0:1], 8), ALU.mult), reads=[Mkb_, gscb], writes=[Gtb])
    P.op("dve", lambda e: e.tensor_tensor(exclm, mtmp, zbc(gsc[:, :, 1:2], 8), ALU.mult), reads=[mtmpb, gscb], writes=[exclmb])
    P.op("dve", lambda e: e.tensor_tensor(Gt, Gt, exclm, ALU.add), reads=[exclmb], writes=[Gtb])
    P.op("dve", lambda e: e.tensor_tensor(Mk, Mk, mtmp, ALU.add), reads=[mtmpb], writes=[Mkb_])
    P.op("dve", lambda e: e.tensor_copy(Mkh, Mk), reads=[Mkb_], writes=[Mkhb])
    for li in range(16):
        bk = rr(C, "pfbank", [0, 1, 2, 3])

        def pf(e, li=li, bk=bk):
            for i in range(li + 1):
                lhs = C.onesb if i < li else C.triub
                ins = e.matmul(ps[:, bk, 0:8], lhs, Mkh[:, i, :], start=(i == 0), stop=(i == li))
            return ins
        P.op("pe", pf, reads=[Mkhb, C.b_onesb, C.b_triub], writes=[C.bank[bk]])
        P.op("act", lambda e, li=li, bk=bk: e.activation(excl[:, li, :], ps[:, bk, 0:8], AF.Copy), reads=[C.bank[bk]], writes=[exclb])
    P.op("dve", lambda e: e.scalar_tensor_tensor(exclm, excl, 1.0, Mk, ALU.add, ALU.mult), reads=[exclb, Mkb_], writes=[exclmb])
    P.op("dve", lambda e: e.tensor_scalar(exclm, exclm, -1.0, None, ALU.add), reads=[exclmb], writes=[exclmb])
    I32 = mybir.dt.int32
    tkf, tkfb = A.alloc([16, 3], F32, name="tkf")
    P.op("pool", lambda e: e.iota(tkf[:, :, 0], [[0, 16]], base=0, channel_multiplier=1, allow_small_or_imprecise_dtypes=True), writes=[tkfb])
    P.op("pool", lambda e: e.iota(tkf[:, :, 1], [[1, 16]], base=0, channel_multiplier=0, allow_small_or_imprecise_dtypes=True), writes=[tkfb])
    P.op("pool", lambda e: e.memset(tkf[:, :, 2], 1.0), writes=[tkfb])
    Gh, Ghb = A.alloc([16, 8], BF16, name="Gh")
    Gm, Gmb = A.alloc([16, 8], BF16, name="Gm")
    Gl, Glb = A.alloc([16, 8], BF16, name="Gl")
    gr, grb = A.alloc([16, 8], F32, name="gr")
    gr2, gr2b = A.alloc([16, 8], F32, name="gr2")
    P.op("dve", lambda e: e.tensor_copy(Gh, Gt), reads=[Gtb], writes=[Ghb])
    P.op("dve", lambda e: e.tensor_copy(gr2, Gh), reads=[Ghb], writes=[gr2b])
    P.op("dve", lambda e: e.tensor_tensor(gr, Gt, gr2, ALU.subtract), reads=[Gtb, gr2b], writes=[grb])
    P.op("dve", lambda e: e.tensor_copy(Gm, gr), reads=[grb], writes=[Gmb])
    P.op("dve", lambda e: e.tensor_copy(gr2, Gm), reads=[Gmb], writes=[gr2b])
    P.op("dve", lambda e: e.tensor_tensor(gr, gr, gr2, ALU.subtract), reads=[gr2b], writes=[grb])
    P.op("dve", lambda e: e.tensor_copy(Gl, gr), reads=[grb], writes=[Glb])
    tkr = Ring(A, 3, [16, 6], BF16, "tk6")
    selr = Ring(A, 3, [16, CAPU], BF16, "sel", nbufs=16)
    hslr = Ring(A, 3, [NSL, 2048], BF16, "hslot", nbufs=NSL)
    hter = Ring(A, 2, [16, CAPU], BF16, "hte", nbufs=16)
    idr = Ring(A, 3, [48], F32, "idxf")
    idir = Ring(A, 3, [8], I32, "idxi")
    for e_ in range(8):
        sel, selb = selr.next()
        for li in range(16):
            P.op("dve", lambda e, sel=sel, li=li, e_=e_: e.tensor_scalar(sel[:, li, :], C.iorow[:, 0:CAPU], excl[:, li, e_:e_ + 1], Mk[:, li, e_:e_ + 1], ALU.is_equal, ALU.mult),
                 reads=[C.b_iorow, exclb, Mkb_], writes=[selb[li]])
        tk6, tk6b = tkr.next()
        P.op("dve", lambda e, tk6=tk6: e.tensor_copy(tk6[:, :, 0:3], tkf), reads=[tkfb], writes=[tk6b])
        for k_, (gt_, gtb_) in enumerate(((Gh, Ghb), (Gm, Gmb), (Gl, Glb))):
            P.op("dve", lambda e, tk6=tk6, gt_=gt_, k_=k_, e_=e_: e.tensor_copy(tk6[:, :, 3 + k_], gt_[:, :, e_]), reads=[gtb_], writes=[tk6b])
        bk = rr(C, "ixbank", [6, 7])

        def ix(e, sel=sel, bk=bk, tk6=tk6):
            for sl in range(NSL):
                for li in range(16):
                    ins = e.matmul(ps[:, bk, sl * 6:sl * 6 + 6], sel[:, li, sl * 128:(sl + 1) * 128], tk6[:, li, :], start=(li == 0), stop=(li == 15))
            return ins
        P.op("pe", ix, reads=selb + [tk6b], writes=[C.bank[bk]])
        idf, idfb = idr.next()
        idi, idib = idir.next()
        P.op("act", lambda e, idf=idf, bk=bk: e.activation(idf[:, 0:6 * NSL], ps[:, bk, 0:6 * NSL], AF.Copy), reads=[C.bank[bk]], writes=[idfb])
        idv = idf[:, 0:6 * NSL].rearrange("p (s c) -> p s c", c=6)
        tix = idf[:, 32:32 + NSL]
        tsc = idf[:, 40:40 + NSL]
        P.op("dve", lambda e, idv=idv, tix=tix: e.scalar_tensor_tensor(tix, idv[:, :, 1], 128.0, idv[:, :, 0], ALU.mult, ALU.add),
             reads=[idfb], writes=[idfb])
        P.op("dve", lambda e, idi=idi, tix=tix: e.tensor_copy(idi[:, 0:NSL], tix), reads=[idfb], writes=[idib])
        P.op("dve", lambda e, idv=idv, e_=e_: e.tensor_tensor(gsl[:, e_, :], idv[:, :, 3], idv[:, :, 4], ALU.add), reads=[idfb], writes=[gslb])
        P.op("dve", lambda e, idv=idv, e_=e_: e.tensor_tensor(gsl[:, e_, :], gsl[:, e_, :], idv[:, :, 5], ALU.add), reads=[idfb], writes=[gslb])
        P.op("dve", lambda e, tsc=tsc, tix=tix: e.tensor_tensor(tsc, tix, C.iops, ALU.subtract), reads=[idfb, C.b_iops], writes=[idfb])
        P.op("dve", lambda e, tsc=tsc: e.tensor_scalar(tsc, tsc, -2048.0, None, ALU.add), reads=[idfb], writes=[idfb])
        P.op("dve", lambda e, tsc=tsc, idv=idv: e.tensor_tensor(tsc, tsc, idv[:, :, 2], ALU.mult), reads=[idfb], writes=[idfb])
        P.op("dve", lambda e, tsc=tsc: e.tensor_tensor(tsc, tsc, C.iops, ALU.add), reads=[idfb, C.b_iops], writes=[idfb])
        P.op("dve", lambda e, tsc=tsc: e.tensor_scalar(tsc, tsc, 2048.0, None, ALU.add), reads=[idfb], writes=[idfb])
        P.op("dve", lambda e, tsc=tsc, e_=e_: e.tensor_copy(idxs[:, e_, 0:NSL], tsc), reads=[idfb], writes=[idxsb])
        hsl, hslb = hslr.next()
        for sl in range(NSL):
            deps = P._deps([idib] + htokdb, [hslb[sl]])
            i_ = P.dma_i["pool"]
            P.dma_i["pool"] += 1
            k_ = "d_pool_%d" % (i_ % P.NDMA)
            if P.cnt[k_] > 0:
                deps.append((k_, P.cnt[k_]))
            P._need("pool", deps)
            P.cnt[k_] += 16
            sem_ = P.sems[k_]
            P.q["pool"].append(lambda e, sl=sl, sem_=sem_, hsl=hsl, idi=idi: e.indirect_dma_start(
                out=hsl[:, sl, :], out_offset=None, in_=htokd.ap(),
                in_offset=bass.IndirectOffsetOnAxis(ap=idi[:, sl:sl + 1], axis=0)).then_inc(sem_, 16))
            P.nins["pool"] += 1
            P._commit((k_, P.cnt[k_]), [idib] + htokdb, [hslb[sl]])
        hte, hteb = hter.next()
        for fb in range(16):
            bk2 = rr(C, "gtbank", [0, 1, 2, 3, 4, 5])
            pb = ps[:, bk2, :].bitcast(BF16)

            def tr(e, hsl=hsl, fb=fb, pb=pb):
                for sl in range(NSL):
                    ins = e.transpose(pb[:, sl * 128:(sl + 1) * 128], hsl[:, sl, fb * 128:(fb + 1) * 128], C.identb)
                return ins
            P.op("pe", tr, reads=hslb + [C.b_identb], writes=[C.bank[bk2]])
            if fb % 2 == 0:
                P.op("act", lambda e, hte=hte, fb=fb, pb=pb: e.activation(hte[:, fb, :], pb[:, 0:CAPU], AF.Copy), reads=[C.bank[bk2]], writes=[hteb[fb]])
            else:
                P.op("dve", lambda e, hte=hte, fb=fb, pb=pb: e.tensor_copy(hte[:, fb, :], pb[:, 0:CAPU]), reads=[C.bank[bk2]], writes=[hteb[fb]])
        P.dma("sp", hted.ap()[e_], hte.rearrange("p a b -> p (a b)"), reads=hteb, writes=[htedb[e_]])
    A.release(m1)
    wg_all = din(C, "wg", [8, 28, 128, 4096]).ap()
    wu_all = din(C, "wu", [8, 28, 128, 4096]).ap()
    wd_all = din(C, "wd", [8, 16, 128, 7168]).ap()
    NPRE = 4
    gl, ul, dl = [], [], []
    for e_ in range(8):
        for q in range(4):
            for s_ in range(7):
                gl.append((wg_all[e_][q * 7 + s_], 16, 256))
                ul.append((wu_all[e_][q * 7 + s_], 16, 256))
            for cg in range(4):
                dl.append((wd_all[e_][q * 4 + cg], 14, 512))
    gst = WStream(C, SlabRing(A, 2, 16, 256, "gslab"), gl)
    ust = WStream(C, SlabRing(A, 2, 16, 256, "uslab"), ul)
    dst_ = WStream(C, SlabRing(A, 2, 14, 512, "dslab"), dl)
    hTer = Ring(A, 2, [16, CAPU], BF16, "hTe")
    hTes = {}

    def load_hTe(e_):
        hTe, hTeb = hTer.next()
        P.dma("sp", hTe.rearrange("p a b -> p (a b)"), hted.ap()[e_], reads=[htedb[e_]], writes=[hTeb])
        hTes[e_] = (hTe, hTeb)
    load_hTe(0)
    for e_ in range(8):
        me = A.mark()
        hTe, hTeb = hTes[e_]
        if e_ + 1 < 8:
            load_hTe(e_ + 1)
        y, yb_ = A.alloc([NSL, 2048], F32, nbufs=NSL * 4, name="y")
        ybuf = lambda sl, cg: yb_[sl * 4 + cg]
        mf = A.mark()
        hidr = Ring(A, 1, [14, CAP], BF16, "hidq", nbufs=14)
        sgr = Ring(A, 2, [2, H], F32, "esg")
        rhs = lambda kc, g, hTe=hTe: hTe[:, kc, g * H:(g + 1) * H]
        for q in range(4):
            hidq, hidqb = hidr.next()
            if CAPU < CAP and q == 0:
                P.op("dve", lambda e, hidq=hidq: e.memset(hidq[:, :, CAPU:CAP], 0.0), writes=hidqb)
            for s in range(7):
                sg_ = gst.get()
                bg = sg_[1]
                su_ = ust.get()
                bu = su_[1]
                for jj in range(2):
                    j = s * 2 + jj
                    bkg = rr(C, "epair", [0, 2, 4, 6])
                    bku = rr(C, "epair", [0, 2, 4, 6])

                    def up(e, slab, bk, jj=jj, rhs=rhs):
                        for kc in range(16):
                            for g in range(2):
                                ins = e.matmul(ps[:, bk + g, 0:H], slab[:, kc, jj * 128:(jj + 1) * 128], rhs(kc, g), start=(kc == 0), stop=(kc == 15))
                        return ins
                    P.op("pe", lambda e, up=up, slab=sg_[0], bk=bkg: up(e, slab, bk), reads=bg + [hTeb], writes=[C.bank[bkg], C.bank[bkg + 1]])
                    P.op("pe", lambda e, up=up, slab=su_[0], bk=bku: up(e, slab, bk), reads=bu + [hTeb], writes=[C.bank[bku], C.bank[bku + 1]])
                    sg, sgb = sgr.next()
                    P.op("act", lambda e, sg=sg, bkg=bkg: e.activation(sg, ps[:, bkg:bkg + 2, 0:H], AF.Silu), reads=[C.bank[bkg], C.bank[bkg + 1]], writes=[sgb])
                    P.op("dve", lambda e, sg=sg, bku=bku, j=j, hidq=hidq: e.tensor_tensor(hidq[:, j, 0:CAPU].rearrange("p (a b) -> p a b", a=2), sg, ps[:, bku:bku + 2, 0:H], ALU.mult),
                         reads=[sgb, C.bank[bku], C.bank[bku + 1]], writes=[hidqb[j]])
                gst.done()
                ust.done()
            for cg in range(4):
                dsl = dst_.get()
                bd = dsl[1]
                for sl in range(NSL):
                    bk = rr(C, "edbank", [0, 1, 2, 3, 4, 5, 6, 7])
                    mm_unitA(C, bk, lambda kc, sl=sl, hidq=hidq: hidq[:, kc, sl * 128:(sl + 1) * 128], hidqb, dsl[0], bd, 14, 512)
                    yv = y[:, sl, cg * 512:(cg + 1) * 512]
                    if q == 0:
                        P.op("act", lambda e, yv=yv, bk=bk: e.activation(yv, ps[:, bk, :], AF.Copy), reads=[C.bank[bk]], writes=[ybuf(sl, cg)])
                    else:
                        P.op("dve", lambda e, yv=yv, bk=bk: e.tensor_tensor(yv, yv, ps[:, bk, :], ALU.add), reads=[C.bank[bk]], writes=[ybuf(sl, cg)])
                dst_.done()
        A.release(mf)
        for sl in range(NSL):
            yrow = y[:, sl, :]
            yb4 = [ybuf(sl, c) for c in range(4)]
            if sl % 2 == 0:
                P.op("act", lambda e, yrow=yrow, sl=sl, e_=e_: e.activation(yrow, yrow, AF.Copy, scale=gsl[:, e_, sl:sl + 1]), reads=[gslb], writes=yb4)
            else:
                P.op("dve", lambda e, yrow=yrow, sl=sl, e_=e_: e.tensor_scalar(yrow, yrow, gsl[:, e_, sl:sl + 1], None, ALU.mult), reads=[gslb], writes=yb4)
            wr = [accall] + ([b_ for row in accb for b_ in row] if (e_ == 0 and sl == 0) else [])
            deps = P._deps(yb4 + [idxsb], wr)
            i_ = P.dma_i["pool"]
            P.dma_i["pool"] += 1
            k_ = "d_pool_%d" % (i_ % P.NDMA)
            if P.cnt[k_] > 0:
                deps.append((k_, P.cnt[k_]))
            P._need("pool", deps)
            P.cnt[k_] += 16
            sem_ = P.sems[k_]
            P.q["pool"].append(lambda e, yrow=yrow, sl=sl, e_=e_, sem_=sem_: e.indirect_dma_start(
                out=accd.ap(), out_offset=bass.IndirectOffsetOnAxis(ap=idxs[:, e_, sl:sl + 1], axis=0),
                in_=yrow, in_offset=None, compute_op=ALU.add).then_inc(sem_, 16))
            P.nins["pool"] += 1
            P._commit((k_, P.cnt[k_]), yb4 + [idxsb], wr)
        A.release(me)
    for gi in range(16):
        P.dma("sp", out.ap()[gi * 128:(gi + 1) * 128, :], accd.ap()[gi * 128:(gi + 1) * 128, :], reads=[accall], writes=outb[gi])
    A.release(m0)

import math


def host_small(inp):
    f = lambda k: np.asarray(inp[k], np.float32)
    pp = np.zeros((128, NPP), np.float32)
    pp[:, PP_CONVB:PP_CONVB + 8] = f("even_conv_b")[0].reshape(8, 128).T
    pp[:, PP_CNG:PP_CNG + 8] = f("even_cnorm_g")[0].reshape(8, 128).T
    pp[:, PP_CNB:PP_CNB + 8] = f("even_cnorm_b")[0].reshape(8, 128).T
    pp[:, PP_BU:PP_BU + 16] = f("odd_b_u")[0][:2048].reshape(16, 128).T
    pp[:, PP_QG] = f("even_q_norm")[0]
    pp[:, PP_KG] = f("even_k_norm")[0]
    pp[:, PP_N3:PP_N3 + 16] = f("odd_norm_ffn")[0].reshape(16, 128).T
    cw = np.ascontiguousarray(f("even_conv_w")[0].T.reshape(8, 128, 31).transpose(1, 0, 2))
    rows = np.zeros((NROWS, 2048), np.float32)
    rows[R_N0] = f("even_norm_mix")[0]
    rows[R_N1] = f("even_norm_ffn")[0]
    rows[R_N2] = f("odd_norm_mix")[0]
    rows[R_N3] = f("odd_norm_ffn")[0]
    rows[R_BU2] = f("odd_b_u")[0][2048:]
    rows[R_VNG] = f("odd_vnorm_g")[0]
    rows[R_VNB] = f("odd_vnorm_b")[0]
    rows[R_BS] = f("odd_b_s")[0].reshape(-1)
    rows[R_ROUTER:R_ROUTER + 8] = f("odd_router")[0].T
    wsT = np.ascontiguousarray(f("odd_w_s")[0].transpose(2, 0, 1))
    wrfm = np.ascontiguousarray(f("odd_router")[0].reshape(16, 128, 8).transpose(1, 0, 2))
    return {"pp": pp, "cw": cw, "rows": rows, "wsT": wsT, "ohu": make_ohu(), "rel_bias": f("rel_bias"), "wrfm": wrfm}


def t5_bucket(d):
    max_exact = 16
    d = max(d, 0)
    if d < max_exact:
        return d
    lr = np.float32(np.log(np.float32(np.float32(max(d, 1)) / np.float32(max_exact)))) / np.float32(math.log(2048 / max_exact))
    large = max_exact + int(np.float32(np.float32(lr) * np.float32(32 - max_exact)))
    return min(large, 31)


def make_ohu():
    oh = np.zeros((64, 3 * 384), np.float32)
    for b, dil in enumerate(DILS):
        for mm in range(384):
            dm = mm - 127
            if 0 <= dm <= 128:
                oh[t5_bucket(dm * dil), b * 384 + mm] = 1.0
            else:
                oh[32, b * 384 + mm] = 1.0
    return oh


def build(upto=99, debug=False):
    nc = bass.Bass("TRN2", target_bir_lowering=False)
    st = ExitStack()
    with st:
        C = setup(nc, st)
        A, P = C.A, C.P
        x = din(C, "x", [2048, 2048])
        out = nc.dram_tensor("out", [2048, 2048], F32, kind="ExternalOutput")
        outb = [[Buf("out%d_%d" % (i, j)) for j in range(4)] for i in range(16)]
        xb_in = Buf("x")
        vd = nc.dram_tensor("vd", [2048, 1024], BF16)
        vdb = [Buf("vd%d" % i) for i in range(16)]
        hTd = nc.dram_tensor("hTd", [2, 128, 16 * 1024], BF16)
        hTdb = [Buf("hTd0"), Buf("hTd1")]
        consts(C)
        m0 = A.mark()
        ob, obb = A.alloc([8, 2048], BF16, nbufs=8, name="ob")
        tails, tailsb = A.alloc([8, 30], BF16, name="tails")
        x_ap = lambda gi: x.ap()[gi * 128:(gi + 1) * 128, :]
        x_bufs = lambda gi: [xb_in]
        for h in range(2):
            m = A.mark()
            hT, hTb = A.alloc([16, 1024], BF16, nbufs=8, name="hT")
            phase_norm(C, x_ap, x_bufs, R_N0, list(range(h * 8, h * 8 + 8)), hT, hTb)
            P.dma("sp", hTd.ap()[h], hT.rearrange("p a b -> p (a b)"), reads=hTb, writes=[hTdb[h]])
            phase_conv(C, h, hT, hTb, ob, obb, tails, tailsb)
            A.release(m)
        qkT, qkb = A.alloc([16, 2048], BF16, nbufs=16, name="qkT")
        for h in range(2):
            m = A.mark()
            hT, hTb1 = A.alloc([16, 1024], BF16, name="hT")
            hTb = [hTb1] * 8
            P.dma("sp", hT.rearrange("p a b -> p (a b)"), hTd.ap()[h], reads=[hTdb[h]], writes=[hTb1])
            phase_v(C, h, hT, hTb, vd, vdb)
            phase_qk(C, h, hT, hTb, qkT, qkb)
            A.release(m)
        m = A.mark()
        biasT, biasb = A.alloc([24, 256], BF16, name="biasT")
        phase_bias(C, biasT, biasb)
        phase_attn(C, qkT, qkb, vd, vdb, biasT, biasb)
        A.release(m)
        lhs = lambda kc, li: (qkT[:, kc, li * 128:(li + 1) * 128] if kc < 8 else ob[:, kc - 8, li * 128:(li + 1) * 128])
        lb = lambda li: qkb[0:8] + obb
        phase_outproj(C, "w_out", [2048, 2048], 16, lhs, lb, list(range(16)),
                      lambda gi, cg: x.ap()[gi * 128:(gi + 1) * 128, cg * 512:(cg + 1) * 512], lambda gi, cg: [xb_in],
                      lambda gi, cg: out.ap()[gi * 128:(gi + 1) * 128, cg * 512:(cg + 1) * 512], lambda gi, cg: [outb[gi][cg]])
        A.release(m0)
        if upto >= 2:
            layer0_ffn(C, out, outb)
        if upto >= 3:
            layer1_mixer(C, out, outb)
        if upto >= 4:
            layer1_moe(C, out, outb)
        P.wait_all("sp", [b for row in outb for b in row])
        print("arena peak", A.peak, "instr counts", P.nins, "sem counts", {k: v for k, v in P.cnt.items() if v})
        P.emit()
        nc._din_names = list(C.din.keys())
    return nc


_WMAP = {"w_in": "even_w_in", "w_out": "even_w_out", "w1": "even_ffn_w1", "w3": "even_ffn_w3", "w2": "even_ffn_w2",
         "w_u": "odd_w_u", "w_o": "odd_w_o"}


def tile_expert_weights(inputs):
    wg = np.asarray(inputs["odd_we_gate"], np.float32)[0]
    wu = np.asarray(inputs["odd_we_up"], np.float32)[0]
    wd = np.asarray(inputs["odd_we_down"], np.float32)[0]
    up = lambda w: np.ascontiguousarray(w.reshape(8, 16, 128, 28, 256).transpose(0, 3, 2, 1, 4)).reshape(8, 28, 128, 4096)
    dn = np.ascontiguousarray(wd.reshape(8, 4, 14, 128, 4, 512).transpose(0, 1, 4, 3, 2, 5)).reshape(8, 16, 128, 7168)
    return {"wg": up(wg), "wu": up(wu), "wd": dn}


def kernel(**inputs):
    n = 8
    nc = build(upto=4)
    small = host_small(inputs)
    shared = dict(small)
    for k, v in _WMAP.items():
        shared[k] = np.asarray(inputs[v], np.float32)[0]
    shared.update(tile_expert_weights(inputs))
    x = np.asarray(inputs["x"], np.float32)
    in_maps = []
    for c in range(n):
        m = {k: shared[k] for k in nc._din_names if k != "x"}
        m["x"] = x[c]
        in_maps.append({k: m[k] for k in nc._din_names})
    res = run_bass_kernel_spmd(nc, in_maps, core_ids=list(range(n)))
    return np.stack([res.results[c]["out"] for c in range(n)], axis=0).astype(np.float32)
```

```python
import numpy as np
from contextlib import ExitStack
import concourse.bass as bass
import concourse.mybir as mybir
from concourse.bass_utils import run_bass_kernel_spmd

F32 = mybir.dt.float32
BF16 = mybir.dt.bfloat16
AF = mybir.ActivationFunctionType
ALU = mybir.AluOpType

T = 2048
D = 2048
EPS = 1e-6
CAP = 640
CAPU = 640
NSL = CAP // 128


class Buf:
    __slots__ = ("name", "w", "r")

    def __init__(self, name=""):
        self.name = name
        self.w = None
        self.r = {}


def _merge(d, k, v):
    if d.get(k, 0) < v:
        d[k] = v


class Prog:
    ENGS = ("pe", "act", "dve", "pool", "sp")
    NDMA = 8

    def __init__(self, nc, stack):
        self.nc = nc
        self.q = {e: [] for e in self.ENGS}
        self.sems = {}
        self.cnt = {}
        for e in ("pe", "act", "dve", "pool"):
            self.sems[e] = stack.enter_context(nc.semaphore("s_" + e))
            self.cnt[e] = 0
        self.dma_i = {}
        for e in ("sp", "act", "pool"):
            for i in range(self.NDMA):
                k = "d_%s_%d" % (e, i)
                self.sems[k] = stack.enter_context(nc.semaphore(k))
                self.cnt[k] = 0
            self.dma_i[e] = 0
        self.waited = {e: {} for e in self.ENGS}
        self.nins = {e: 0 for e in self.ENGS}

    def _need(self, eng, deps):
        best = {}
        for d in deps:
            if d is None:
                continue
            k, v = d
            if eng == "pe" and k == "pe":
                continue
            _merge(best, k, v)
        for k, v in best.items():
            if self.waited[eng].get(k, 0) >= v:
                continue
            self.waited[eng][k] = v
            sem = self.sems[k]
            self.q[eng].append(lambda e, sem=sem, v=v: e.wait_ge(sem, v))

    @staticmethod
    def _deps(reads, writes):
        deps = []
        for b in reads:
            deps.append(b.w)
        for b in writes:
            deps.append(b.w)
            deps.extend(b.r.items())
        return deps

    @staticmethod
    def _commit(tok, reads, writes):
        k, v = tok
        for b in reads:
            _merge(b.r, k, v)
        for b in writes:
            b.w = tok
            b.r = {}

    def op(self, eng, fn, reads=(), writes=()):
        self._need(eng, self._deps(reads, writes))
        self.cnt[eng] += 1
        sem = self.sems[eng]
        self.q[eng].append(lambda e, fn=fn, sem=sem: fn(e).then_inc(sem, 1))
        self.nins[eng] += 1
        self._commit((eng, self.cnt[eng]), reads, writes)

    def dma(self, qeng, out, in_, reads=(), writes=(), **kw):
        deps = self._deps(reads, writes)
        i = self.dma_i[qeng]
        self.dma_i[qeng] += 1
        k = "d_%s_%d" % (qeng, i % self.NDMA)
        if self.cnt[k] > 0:
            deps.append((k, self.cnt[k]))
        self._need(qeng, deps)
        self.cnt[k] += 16
        sem = self.sems[k]
        self.q[qeng].append(
            lambda e, out=out, in_=in_, sem=sem, kw=kw: e.dma_start(out=out, in_=in_, **kw).then_inc(sem, 16))
        self.nins[qeng] += 1
        self._commit((k, self.cnt[k]), reads, writes)

    def wait_all(self, eng, bufs):
        deps = []
        for b in bufs:
            deps.append(b.w)
            deps.extend(b.r.items())
        self._need(eng, deps)

    def emit(self):
        nc = self.nc
        with nc.Block() as block:
            def mk(name):
                lst = self.q[name]

                def body(e):
                    for f in lst:
                        f(e)
                return body
            if self.q["sp"]:
                block.sync(mk("sp"))
            if self.q["act"]:
                block.scalar(mk("act"))
            if self.q["dve"]:
                block.vector(mk("dve"))
            if self.q["pool"]:
                block.gpsimd(mk("pool"))
            if self.q["pe"]:
                block.tensor(mk("pe"))


class Arena:
    def __init__(self, t, nbytes):
        self.t = t
        self.nbytes = nbytes
        self.top = 0
        self.live = []
        self.dead = []
        self.peak = 0

    def alloc(self, shape, dtype, nbufs=1, name=""):
        esz = 2 if dtype == BF16 else 4
        n = 1
        for s in shape:
            n *= s
        nb = n * esz
        assert nb % 4 == 0
        nb_al = (nb + 63) // 64 * 64
        start = self.top
        end = start + nb_al
        assert end <= self.nbytes, "arena overflow %s: %d > %d" % (name, end, self.nbytes)
        self.top = end
        self.peak = max(self.peak, end)
        ap = self.t[:, start // 4:(start + nb) // 4]
        if dtype != F32:
            ap = ap.bitcast(dtype)
        if len(shape) == 2:
            ap = ap.rearrange("p (a b) -> p a b", a=shape[0])
        elif len(shape) == 3:
            ap = ap.rearrange("p (a b c) -> p a b c", a=shape[0], b=shape[1])
        bufs = [Buf(name) for _ in range(nbufs)]
        keep = []
        for (s, e, toks) in self.dead:
            if s < end and e > start:
                for b in bufs:
                    for k, v in toks.items():
                        _merge(b.r, k, v)
                if s >= start and e <= end:
                    continue
            keep.append((s, e, toks))
        self.dead = keep
        self.live.append((start, end, bufs))
        return ap, (bufs[0] if nbufs == 1 else bufs)

    def mark(self):
        return (self.top, len(self.live))

    def release(self, mark):
        top, n = mark
        for (s, e, bufs) in self.live[n:]:
            toks = {}
            for b in bufs:
                if b.w is not None:
                    _merge(toks, b.w[0], b.w[1])
                for k, v in b.r.items():
                    _merge(toks, k, v)
            self.dead.append((s, e, toks))
        del self.live[n:]
        self.top = top


class Ring:
    def __init__(self, A, n, shape, dtype, name="", nbufs=1):
        self.slots = [A.alloc(shape, dtype, nbufs=nbufs, name="%s%d" % (name, i)) for i in range(n)]
        self.i = 0

    def next(self):
        s = self.slots[self.i % len(self.slots)]
        self.i += 1
        return s


class Ctx:
    pass


ARENA_BYTES = 206 * 1024
DILS = (1, 4, 16)
NBS = (16, 4, 1)
COFF = (0, 16, 32)

PP_CONVB, PP_CNG, PP_CNB, PP_BU, PP_QG, PP_KG, PP_N3, NPP = 0, 8, 16, 24, 40, 41, 42, 58
R_N0, R_N1, R_N2, R_N3, R_BU2, R_VNG, R_VNB, R_BS, R_ROUTER, NROWS = 0, 1, 2, 3, 4, 5, 6, 7, 8, 16


def din(C, name, shape, dtype=F32):
    if name not in C.din:
        C.din[name] = C.nc.dram_tensor(name, list(shape), dtype, kind="ExternalInput")
    return C.din[name]


def setup(nc, st):
    C = Ctx()
    C.nc = nc
    C.P = Prog(nc, st)
    C.din = {}
    C.arena_t = st.enter_context(nc.sbuf_tensor("arena", [128, ARENA_BYTES // 4], F32))
    C.A = Arena(C.arena_t, ARENA_BYTES)
    C.ps = st.enter_context(nc.psum_tensor("ps", [128, 8, 512], F32))
    C.bank = [Buf("bank%d" % i) for i in range(8)]
    C.rr = {}
    return C


def rr(C, key, items):
    i = C.rr.get(key, 0)
    C.rr[key] = i + 1
    return items[i % len(items)]


def consts(C):
    A, P, nc = C.A, C.P, C.nc
    C.identf, C.b_identf = A.alloc([128], F32, name="identf")
    C.identb, C.b_identb = A.alloc([128], BF16, name="identb")
    C.onesf, C.b_onesf = A.alloc([128], F32, name="onesf")
    C.onesb, C.b_onesb = A.alloc([128], BF16, name="onesb")
    C.antif, C.b_antif = A.alloc([128], F32, name="antif")
    C.triub, C.b_triub = A.alloc([128], BF16, name="triub")
    C.iorow, C.b_iorow = A.alloc([CAP], F32, name="iorow")
    C.iops, C.b_iops = A.alloc([NSL], F32, name="iops")
    C.pp, C.b_pp = A.alloc([NPP], F32, name="pp")
    C.cw, C.b_cw = A.alloc([8, 31], F32, name="cw")
    C.gvec, C.b_gvec = A.alloc([16], F32, name="gvec")
    P.op("pool", lambda e: e.memset(C.identf, 1.0), writes=[C.b_identf])
    P.op("pool", lambda e: e.affine_select(C.identf, C.identf, [[-1, 128]], ALU.is_equal, 0.0, base=0, channel_multiplier=1),
         reads=[C.b_identf], writes=[C.b_identf])
    P.op("pool", lambda e: e.memset(C.onesf, 1.0), writes=[C.b_onesf])
    P.op("pool", lambda e: e.memset(C.onesb, 1.0), writes=[C.b_onesb])
    P.op("pool", lambda e: e.memset(C.antif, 1.0), writes=[C.b_antif])
    P.op("pool", lambda e: e.affine_select(C.antif, C.antif, [[1, 128]], ALU.is_equal, 0.0, base=-127, channel_multiplier=1),
         reads=[C.b_antif], writes=[C.b_antif])
    P.op("pool", lambda e: e.memset(C.triub, 1.0), writes=[C.b_triub])
    P.op("pool", lambda e: e.affine_select(C.triub, C.triub, [[1, 128]], ALU.is_ge, 0.0, base=-1, channel_multiplier=-1),
         reads=[C.b_triub], writes=[C.b_triub])
    P.op("pool", lambda e: e.iota(C.iorow, [[1, CAP]], base=0, channel_multiplier=0, allow_small_or_imprecise_dtypes=True),
         writes=[C.b_iorow])
    P.op("pool", lambda e: e.iota(C.iops, [[128, NSL]], base=0, channel_multiplier=1, allow_small_or_imprecise_dtypes=True),
         writes=[C.b_iops])
    P.op("dve", lambda e: e.tensor_copy(C.identb, C.identf), reads=[C.b_identf], writes=[C.b_identb])
    P.dma("sp", C.pp, din(C, "pp", [128, NPP]).ap(), writes=[C.b_pp])
    P.dma("sp", C.cw, din(C, "cw", [128, 8, 31]).ap(), writes=[C.b_cw])
    sc = float(128 ** -0.5)
    P.op("dve", lambda e: e.tensor_scalar(C.gvec[:, 0:8], C.onesf[:, 0:8], C.pp[:, PP_QG:PP_QG + 1], sc, ALU.mult, ALU.mult),
         reads=[C.b_pp, C.b_onesf], writes=[C.b_gvec])
    P.op("dve", lambda e: e.tensor_scalar(C.gvec[:, 8:16], C.onesf[:, 0:8], C.pp[:, PP_KG:PP_KG + 1], None, ALU.mult),
         reads=[C.b_pp, C.b_onesf], writes=[C.b_gvec])


def row_bcast(C, r, n=2048, off=0):
    rows = din(C, "rows", [NROWS, 2048])
    return bass.AP(rows, r * 2048 + off, [[0, 128], [1, n]])


def wview(w_ap):
    return w_ap.rearrange("(kc p) f -> p kc f", p=128)


def load_slab(C, slot, wv, kc0, kc1, c0, w):
    ap, bufs = slot
    i = 0
    for k in range(kc0, kc1, 8):
        k2 = min(k + 8, kc1)
        C.P.dma("pool", ap[:, k - kc0:k2 - kc0, 0:w], wv[:, k:k2, c0:c0 + w], writes=[bufs[i]])
        i += 1
    return bufs[:i]


class SlabRing:
    def __init__(self, A, n, kcn, w, name="slab"):
        self.slots = [A.alloc([kcn, w], BF16, nbufs=(kcn + 7) // 8, name="%s%d" % (name, i)) for i in range(n)]
        self.slots = [(ap, b if isinstance(b, list) else [b]) for ap, b in self.slots]
        self.i = 0

    def next(self):
        s = self.slots[self.i % len(self.slots)]
        self.i += 1
        return s


def phase_norm(C, src_ap, src_bufs, row, tiles, hT, hTb, htok=None, htokb=None, post=None, tokout=None):
    A, P = C.A, C.P
    m = A.mark()
    gbc, gb = A.alloc([2048], F32, name="gbc")
    P.dma("sp", gbc, row_bcast(C, row), writes=[gb])
    xin = Ring(A, 2, [2048], F32, "xin")
    hbr = Ring(A, 2, [2048], BF16, "hb")
    junk, jb = A.alloc([2048], BF16, name="junk")
    nt = len(tiles)
    ss, ssb = A.alloc([nt], F32, nbufs=nt, name="ss")
    rs, rsb = A.alloc([nt], F32, nbufs=nt, name="rs")
    if nt == 1:
        ssb, rsb = [ssb], [rsb]
    for li, gi in enumerate(tiles):
        xt, xb = xin.next()
        P.dma("sp", xt, src_ap(gi), reads=src_bufs(gi), writes=[xb])
        P.op("act", lambda e, xt=xt, li=li: e.activation(junk, xt, AF.Square, accum_out=ss[:, li:li + 1]),
             reads=[xb], writes=[jb, ssb[li]])
        P.op("act", lambda e, li=li: e.activation(rs[:, li:li + 1], ss[:, li:li + 1], AF.Sqrt, scale=1.0 / D, bias=EPS),
             reads=[ssb[li]], writes=[rsb[li]])
        P.op("dve", lambda e, li=li: e.reciprocal(rs[:, li:li + 1], rs[:, li:li + 1]), reads=[rsb[li]], writes=[rsb[li]])
        if htok is None:
            hb, hbb = hbr.next()
        else:
            hb, hbb = htok[:, li, :], htokb[li]
        if post is not None:
            post(li, gi, xt, xb, rs[:, li:li + 1], rsb[li], gbc, gb)
        P.op("dve", lambda e, hb=hb, xt=xt, li=li: e.scalar_tensor_tensor(hb, xt, rs[:, li:li + 1], gbc, ALU.mult, ALU.mult),
             reads=[xb, rsb[li], gb], writes=[hbb])
        if tokout is not None:
            tokout(li, gi, hb, hbb)
        if hT is None:
            continue
        for half in range(2):
            bk = rr(C, "tbank", [4, 5, 6, 7])
            pb = C.ps[:, bk, :].bitcast(BF16)

            def tr(e, hb=hb, pb=pb, half=half):
                for k in range(8):
                    kc = half * 8 + k
                    ins = e.transpose(pb[:, k * 128:(k + 1) * 128], hb[:, kc * 128:(kc + 1) * 128], C.identb)
                return ins
            P.op("pe", tr, reads=[hbb, C.b_identb], writes=[C.bank[bk]])
            dst = hT[:, half * 8:(half + 1) * 8, li * 128:(li + 1) * 128]
            src = pb.rearrange("p (k t) -> p k t", k=8)
            if half == 0:
                P.op("act", lambda e, dst=dst, src=src: e.activation(dst, src, AF.Copy), reads=[C.bank[bk]], writes=[hTb[li]])
            else:
                P.op("dve", lambda e, dst=dst, src=src: e.tensor_copy(dst, src), reads=[C.bank[bk]], writes=[hTb[li]])
    A.release(m)


def mm_unitB(C, bk, slab, sbufs, col, kcn, rhs_fn, rbufs, ng=2):
    ps = C.ps

    def fn(e):
        for kc in range(kcn):
            for g in range(ng):
                ins = e.matmul(ps[:, bk + g, :], slab[:, kc, col:col + 128], rhs_fn(kc, g), start=(kc == 0), stop=(kc == kcn - 1))
        return ins
    C.P.op("pe", fn, reads=list(sbufs) + list(rbufs), writes=[C.bank[bk + g] for g in range(ng)])


def mm_unitA(C, bk, lhs_fn, lbufs, slab, sbufs, kcn, w, kc_off=0, start=True, stop=True):
    ps = C.ps

    def fn(e):
        for kc in range(kcn):
            ins = e.matmul(ps[:, bk, 0:w], lhs_fn(kc_off + kc), slab[:, kc, 0:w], start=(start and kc == 0), stop=(stop and kc == kcn - 1))
        return ins
    C.P.op("pe", fn, reads=list(sbufs) + list(lbufs), writes=[C.bank[bk]])


def phase_conv(C, h, hT, hTb, ob, obb, tails, tailsb):
    A, P, ps = C.A, C.P, C.ps
    m = A.mark()
    w_in = wview(din(C, "w_in", [2048, 5120]).ap())
    slabs = SlabRing(A, 4, 16, 256, "cslab")
    cc, ccb = A.alloc([8, 1024], F32, nbufs=8, name="cc")
    glur = Ring(A, 2, [1056], BF16, "glu")
    diagr = Ring(A, 2, [31, 128], BF16, "cdiag")
    idrep, idrepb = A.alloc([31, 128], BF16, name="idrep")
    sigr = Ring(A, 2, [1024], F32, "sig")
    sqf, sqfb = A.alloc([1024], F32, name="sqf")
    for k in range(31):
        P.op("dve", lambda e, k=k: e.tensor_copy(idrep[:, k, :], C.identb), reads=[C.b_identb], writes=[idrepb])
    rhs = lambda kc, g: hT[:, kc, g * 512:(g + 1) * 512]
    flat = lambda b0: ps[:, b0:b0 + 2, :].rearrange("p a b -> p (a b)")
    for cp in range(4):
        sv = slabs.next()
        bv = load_slab(C, sv, w_in, 0, 16, 3072 + cp * 256, 256)
        sg = slabs.next()
        bg = load_slab(C, sg, w_in, 0, 16, 4096 + cp * 256, 256)
        for c2 in range(2):
            c = cp * 2 + c2
            mm_unitB(C, 0, sv[0], bv, c2 * 128, 16, rhs, hTb)
            mm_unitB(C, 2, sg[0], bg, c2 * 128, 16, rhs, hTb)
            sig, sigb = sigr.next()
            P.op("act", lambda e, sig=sig: e.activation(sig, flat(2), AF.Sigmoid), reads=[C.bank[2], C.bank[3]], writes=[sigb])
            glu, glub = glur.next()
            if h == 0:
                P.op("dve", lambda e, glu=glu: e.memset(glu[:, 0:30], 0.0), writes=[glub])
            else:
                P.op("dve", lambda e, glu=glu, c=c: e.tensor_copy(glu[:, 0:30], tails[:, c, :]), reads=[tailsb], writes=[glub])
            P.op("dve", lambda e, glu=glu, sig=sig: e.tensor_tensor(glu[:, 30:1054], flat(0), sig, ALU.mult),
                 reads=[C.bank[0], C.bank[1], sigb], writes=[glub])
            if h == 0:
                P.op("dve", lambda e, glu=glu, c=c: e.tensor_copy(tails[:, c, :], glu[:, 1024:1054]), reads=[glub], writes=[tailsb])
            dg, dgb = diagr.next()
            a0 = C.cw[:, c, :]
            cwb = bass.AP(a0.tensor, a0.offset, [list(a0.ap[0]), [1, 31], [0, 128]])
            P.op("dve", lambda e, dg=dg, cwb=cwb: e.tensor_tensor(dg, idrep, cwb, ALU.mult), reads=[idrepb, C.b_cw], writes=[dgb])
            bk = rr(C, "cvpair", [4, 6])

            def conv(e, dg=dg, glu=glu, bk=bk):
                for g in range(2):
                    for k in range(31):
                        ins = e.matmul(ps[:, bk + g, :], dg[:, k, :], glu[:, k + g * 512:k + g * 512 + 512], start=(k == 0), stop=(k == 30))
                return ins
            P.op("pe", conv, reads=[dgb, glub], writes=[C.bank[bk], C.bank[bk + 1]])
            P.op("act", lambda e, c=c, bk=bk: e.activation(cc[:, c, :], flat(bk), AF.Identity, bias=C.pp[:, PP_CONVB + c:PP_CONVB + c + 1]),
                 reads=[C.bank[bk], C.bank[bk + 1], C.b_pp], writes=[ccb[c]])
    for c in range(8):
        accA = cc[:, c, :]

        def st1(e, accA=accA, c=c):
            for g in range(2):
                ins = e.matmul(ps[:, g, :], C.onesf, accA[:, g * 512:(g + 1) * 512], start=(c == 0), stop=(c == 7))
            return ins
        P.op("pe", st1, reads=[ccb[c], C.b_onesf], writes=[C.bank[0], C.bank[1]])
        P.op("act", lambda e, accA=accA: e.activation(sqf, accA, AF.Square), reads=[ccb[c]], writes=[sqfb])

        def st2(e, c=c):
            for g in range(2):
                ins = e.matmul(ps[:, 2 + g, :], C.onesf, sqf[:, g * 512:(g + 1) * 512], start=(c == 0), stop=(c == 7))
            return ins
        P.op("pe", st2, reads=[sqfb, C.b_onesf], writes=[C.bank[2], C.bank[3]])
    meant, meanb = A.alloc([1024], F32, name="mean")
    vart, varb = A.alloc([1024], F32, name="var")
    rstd, rstdb = A.alloc([1024], F32, name="rstd")
    yr = Ring(A, 2, [1024], F32, "y")
    P.op("act", lambda e: e.activation(meant, flat(0), AF.Copy, scale=1.0 / 1024), reads=[C.bank[0], C.bank[1]], writes=[meanb])
    P.op("dve", lambda e: e.tensor_tensor(vart, meant, meant, ALU.mult), reads=[meanb], writes=[varb])
    P.op("dve", lambda e: e.scalar_tensor_tensor(vart, flat(2), 1.0 / 1024, vart, ALU.mult, ALU.subtract),
         reads=[C.bank[2], C.bank[3]], writes=[varb])
    P.op("act", lambda e: e.activation(rstd, vart, AF.Sqrt, bias=EPS), reads=[varb], writes=[rstdb])
    P.op("dve", lambda e: e.reciprocal(rstd, rstd), reads=[rstdb], writes=[rstdb])
    for c in range(8):
        y, yb = yr.next()
        P.op("dve", lambda e, y=y, c=c: e.tensor_tensor(y, cc[:, c, :], meant, ALU.subtract), reads=[ccb[c], meanb], writes=[yb])
        P.op("dve", lambda e, y=y: e.tensor_tensor(y, y, rstd, ALU.mult), reads=[rstdb], writes=[yb])
        P.op("act", lambda e, y=y, c=c: e.activation(ob[:, c, h * 1024:(h + 1) * 1024], y, AF.Silu,
                                                      scale=C.pp[:, PP_CNG + c:PP_CNG + c + 1], bias=C.pp[:, PP_CNB + c:PP_CNB + c + 1]),
             reads=[yb, C.b_pp], writes=[obb[c]])
    A.release(m)


def phase_v(C, h, hT, hTb, vd, vdb):
    A, P, ps = C.A, C.P, C.ps
    m = A.mark()
    w_in = wview(din(C, "w_in", [2048, 5120]).ap())
    slabs = SlabRing(A, 3, 16, 256, "vslab")
    vtok, vtokb = A.alloc([8, 1024], BF16, nbufs=8, name="vtok")
    for s4 in range(4):
        sl = slabs.next()
        sb = load_slab(C, sl, w_in, 0, 16, 2048 + s4 * 256, 256)
        for li in range(8):
            bk = rr(C, "vbank", [0, 1, 2, 3])
            mm_unitA(C, bk, lambda kc, li=li: hT[:, kc, li * 128:(li + 1) * 128], [hTb[li]], sl[0], sb, 16, 256)
            dst = vtok[:, li, s4 * 256:(s4 + 1) * 256]
            if li % 2 == 0:
                P.op("act", lambda e, dst=dst, bk=bk: e.activation(dst, ps[:, bk, 0:256], AF.Copy), reads=[C.bank[bk]], writes=[vtokb[li]])
            else:
                P.op("dve", lambda e, dst=dst, bk=bk: e.tensor_copy(dst, ps[:, bk, 0:256]), reads=[C.bank[bk]], writes=[vtokb[li]])
    for li in range(8):
        gi = h * 8 + li
        P.dma("sp", vd.ap()[gi * 128:(gi + 1) * 128, :], vtok[:, li, :], reads=[vtokb[li]], writes=[vdb[gi]])
    A.release(m)


def phase_qk(C, h, hT, hTb, qkT, qkb):
    A, P, ps = C.A, C.P, C.ps
    m = A.mark()
    w_in = wview(din(C, "w_in", [2048, 5120]).ap())
    slabs = SlabRing(A, 3, 16, 256, "qslab")
    rawr = Ring(A, 2, [1024], F32, "raw")
    sqr = Ring(A, 2, [1024], BF16, "sq")
    rtr = Ring(A, 2, [1024], F32, "rt")
    rhs = lambda kc, g: hT[:, kc, g * 512:(g + 1) * 512]
    flat = lambda b0: ps[:, b0:b0 + 2, :].rearrange("p a b -> p (a b)")
    for s in range(8):
        sl = slabs.next()
        sb = load_slab(C, sl, w_in, 0, 16, s * 256, 256)
        for jj in range(2):
            j = s * 2 + jj
            bk = rr(C, "qkpair", [0, 2, 4])
            mm_unitB(C, bk, sl[0], sb, jj * 128, 16, rhs, hTb)
            raw, rawb = rawr.next()
            sq, sqb = sqr.next()
            rt, rtb = rtr.next()
            P.op("act", lambda e, raw=raw, bk=bk: e.activation(raw, flat(bk), AF.Copy), reads=[C.bank[bk], C.bank[bk + 1]], writes=[rawb])
            P.op("act", lambda e, sq=sq, bk=bk: e.activation(sq, flat(bk), AF.Square), reads=[C.bank[bk], C.bank[bk + 1]], writes=[sqb])

            def ssq(e, sq=sq):
                for g in range(2):
                    ins = e.matmul(ps[:, 6 + g, :], C.onesb, sq[:, g * 512:(g + 1) * 512], start=True, stop=True)
                return ins
            P.op("pe", ssq, reads=[sqb, C.b_onesb], writes=[C.bank[6], C.bank[7]])
            P.op("act", lambda e, rt=rt: e.activation(rt, flat(6), AF.Sqrt, scale=1.0 / 128, bias=EPS), reads=[C.bank[6], C.bank[7]], writes=[rtb])
            P.op("dve", lambda e, rt=rt: e.reciprocal(rt, rt), reads=[rtb], writes=[rtb])
            P.op("dve", lambda e, raw=raw, rt=rt, j=j: e.scalar_tensor_tensor(qkT[:, j, h * 1024:(h + 1) * 1024], raw, C.gvec[:, j:j + 1], rt, ALU.mult, ALU.mult),
                 reads=[rawb, rtb, C.b_gvec], writes=[qkb[j]])
    A.release(m)


def phase_bias(C, biasT, biasb):
    A, P, ps, nc = C.A, C.P, C.ps, C.nc
    m = A.mark()
    rb, rbb = A.alloc([8], F32, name="rbaug")
    oh, ohb = A.alloc([1152], F32, name="ohu")
    usb, usbb = A.alloc([1152], F32, name="usb")
    hs, hsb = A.alloc([8, 256], F32, name="hs")
    ud = nc.dram_tensor("ud", [8, 1152], F32)
    udb = Buf("ud")
    P.op("dve", lambda e: e.memset(rb[0:64, :], -30000.0), writes=[rbb])
    P.dma("sp", rb[0:32, :], din(C, "rel_bias", [32, 8]).ap(), writes=[rbb])
    P.dma("sp", oh[0:64, :], din(C, "ohu", [64, 1152]).ap(), writes=[ohb])
    for b in range(3):
        P.op("pe", lambda e, b=b: e.matmul(ps[0:8, b, 0:384], rb[0:64, :], oh[0:64, b * 384:(b + 1) * 384], start=True, stop=True),
             reads=[rbb, ohb], writes=[C.bank[b]])
        P.op("act", lambda e, b=b: e.activation(usb[0:8, b * 384:(b + 1) * 384], ps[0:8, b, 0:384], AF.Copy), reads=[C.bank[b]], writes=[usbb])
    P.dma("sp", ud.ap(), usb[0:8, :], reads=[usbb], writes=[udb])
    for b in range(3):
        src = bass.AP(ud, b * 384, [[1, 128], [1152, 8], [1, 256]])
        P.dma("sp", hs, src, reads=[udb], writes=[hsb])
        for hd in range(8):
            bk = rr(C, "bbank", [3, 4, 5, 6])
            P.op("pe", lambda e, hd=hd, bk=bk: e.matmul(ps[:, bk, 0:256], C.antif, hs[:, hd, :], start=True, stop=True),
                 reads=[hsb, C.b_antif], writes=[C.bank[bk]])
            P.op("act", lambda e, b=b, hd=hd, bk=bk: e.activation(biasT[:, b * 8 + hd, :], ps[:, bk, 0:256], AF.Copy),
                 reads=[C.bank[bk]], writes=[biasb])
    A.release(m)


def phase_attn(C, qkT, qkb, vd, vdb, biasT, biasb):
    A, P, ps = C.A, C.P, C.ps
    m = A.mark()
    vhr = Ring(A, 2, [48, 128], BF16, "vh", nbufs=21)
    od, odb = A.alloc([2, 2048], F32, name="od")
    ptr = Ring(A, 8, [256], BF16, "pt")
    LOOK = 2
    for hd in range(8):
        vh, vhbl = vhr.next()
        vhbm = {}
        ib = 0
        for b in range(3):
            dil, nb = DILS[b], NBS[b]
            for r in range(dil):
                src = bass.AP(vd, r * 1024 + hd * 128, [[dil * 1024, 128], [128 * dil * 1024, nb], [1, 128]])
                c0 = COFF[b] + r * nb
                P.dma("sp", vh[:, c0:c0 + nb, :], src, reads=vdb, writes=[vhbl[ib]])
                vhbm[(b, r)] = vhbl[ib]
                ib += 1
        combos = [(b, r, n) for b in range(3) for r in range(DILS[b]) for n in range(NBS[b])]
        pts = {}

        def stage_a(i, hd=hd):
            b, r, n = combos[i]
            dil, nb = DILS[b], NBS[b]
            start = n * 128 * dil + r
            nq = 256 if n < nb - 1 else 128
            kT = qkT[:, 8 + hd, start:start + 127 * dil + 1:dil]
            qT = qkT[:, hd, start:start + (nq - 1) * dil + 1:dil]
            bk = rr(C, "sbank", [0, 1, 2, 3])

            def sc(e, bk=bk, kT=kT, qT=qT, nq=nq, b=b):
                e.matmul(ps[:, bk, 0:nq], kT, qT, start=True, stop=False)
                return e.matmul(ps[:, bk, 0:nq], C.identb, biasT[:, b * 8 + hd, 0:nq], start=False, stop=True)
            P.op("pe", sc, reads=[qkb[hd], qkb[8 + hd], biasb, C.b_identb], writes=[C.bank[bk]])
            p_t, p_b = ptr.next()
            P.op("act", lambda e, bk=bk, p_t=p_t, nq=nq: e.activation(p_t[:, 0:nq], ps[:, bk, 0:nq], AF.Exp), reads=[C.bank[bk]], writes=[p_b])
            pts[i] = (p_t, p_b)

        def stage_b(i, hd=hd, vh=vh):
            b, r, n = combos[i]
            dil, nb = DILS[b], NBS[b]
            start = n * 128 * dil + r
            p_t, p_b = pts[i]
            prev = pts[i - 1] if n > 0 else None
            c = COFF[b] + r * nb + n
            ob_ = rr(C, "obank", [4, 5, 6, 7])

            def pv(e, ob_=ob_, prev=prev, p_t=p_t, c=c, vh=vh):
                for grp in range(2):
                    lhs_prev = (vh[:, c - 1, :] if grp == 0 else C.onesb) if prev is not None else None
                    lhs_cur = vh[:, c, :] if grp == 0 else C.onesb
                    o = ps[:, ob_, grp * 128:(grp + 1) * 128]
                    if prev is not None:
                        e.matmul(o, lhs_prev, prev[0][:, 128:256], start=True, stop=False)
                    ins = e.matmul(o, lhs_cur, p_t[:, 0:128], start=(prev is None), stop=True)
                return ins
            rd = [vhbm[(b, r)], p_b, C.b_onesb] + ([prev[1]] if prev is not None else [])
            P.op("pe", pv, reads=rd, writes=[C.bank[ob_]])
            odv = od[:, :, start:start + 127 * dil + 1:dil]
            psv = ps[:, ob_, 0:256].rearrange("p (a t) -> p a t", a=2)
            if b == 0:
                P.op("act", lambda e, odv=odv, psv=psv: e.activation(odv, psv, AF.Copy), reads=[C.bank[ob_]], writes=[odb])
            else:
                P.op("dve", lambda e, odv=odv, psv=psv: e.tensor_tensor(odv, odv, psv, ALU.add), reads=[C.bank[ob_]], writes=[odb])
            if i - 1 in pts and (n == 0 or True):
                pass
        N = len(combos)
        for i in range(N + LOOK):
            if i < N:
                stage_a(i)
            if i - LOOK >= 0:
                stage_b(i - LOOK)
        P.op("dve", lambda e: e.reciprocal(od[:, 1, :], od[:, 1, :]), reads=[odb], writes=[odb])
        P.op("dve", lambda e, hd=hd: e.tensor_tensor(qkT[:, hd, :], od[:, 0, :], od[:, 1, :], ALU.mult), reads=[odb], writes=[qkb[hd]])
    A.release(m)


def phase_outproj(C, wname, wshape, kcn, lhs_fn, lbufs_fn, tiles, res_ap, res_bufs, dst_ap, dst_bufs, kslab=None):
    A, P, ps = C.A, C.P, C.ps
    m = A.mark()
    wv = wview(din(C, wname, wshape).ap())
    kslab = kslab or kcn
    nks = kcn // kslab
    slabs = SlabRing(A, 3 if kslab <= 16 else 2, kslab, 512, "oslab")
    resr = Ring(A, 5, [512], F32, "res")
    nt = len(tiles)
    for cg in range(4):
        if nks == 1:
            sl = slabs.next()
            sb = load_slab(C, sl, wv, 0, kcn, cg * 512, 512)
            for li, gi in enumerate(tiles):
                bk = rr(C, "opbank", [0, 1, 2, 3, 4, 5, 6, 7])
                mm_unitA(C, bk, lambda kc, li=li: lhs_fn(kc, li), lbufs_fn(li), sl[0], sb, kcn, 512)
                _op_evac(C, resr, bk, gi, cg, res_ap, res_bufs, dst_ap, dst_bufs)
        else:
            assert nt <= 8
            for ks in range(nks):
                sl = slabs.next()
                sb = load_slab(C, sl, wv, ks * kslab, (ks + 1) * kslab, cg * 512, 512)
                for li, gi in enumerate(tiles):
                    mm_unitA(C, li, lambda kc, li=li: lhs_fn(kc, li), lbufs_fn(li), sl[0], sb, kslab, 512,
                             kc_off=ks * kslab, start=(ks == 0), stop=(ks == nks - 1))
                    if ks == nks - 1:
                        _op_evac(C, resr, li, gi, cg, res_ap, res_bufs, dst_ap, dst_bufs)
    A.release(m)


def _op_evac(C, resr, bk, gi, cg, res_ap, res_bufs, dst_ap, dst_bufs):
    P, ps = C.P, C.ps
    rt, rtb = resr.next()
    P.dma("act", rt, res_ap(gi, cg), reads=res_bufs(gi, cg), writes=[rtb])
    P.op("dve", lambda e, rt=rt, bk=bk: e.tensor_tensor(rt, rt, ps[:, bk, :], ALU.add), reads=[C.bank[bk]], writes=[rtb])
    P.dma("sp", dst_ap(gi, cg), rt, reads=[rtb], writes=dst_bufs(gi, cg))


GELU_C = 0.044715
GELU_K = 1.5957691216057308


def flat2(C, bk, n=512):
    if n == 512:
        return C.ps[:, bk:bk + 2, :].rearrange("p a b -> p (a b)")
    return C.ps[:, bk:bk + 2, 0:n]


def out_tile_ap(out, gi, c0, w):
    return out.ap()[gi * 128:(gi + 1) * 128, c0:c0 + w]


def layer0_ffn(C, out, outb):
    A, P, ps = C.A, C.P, C.ps
    w1 = wview(din(C, "w1", [2048, 5632]).ap())
    w3 = wview(din(C, "w3", [2048, 5632]).ap())
    for h in range(2):
        tiles = list(range(h * 8, h * 8 + 8))
        m = A.mark()
        hid, hidb = A.alloc([44, 1024], BF16, nbufs=44, name="hid")
        m2 = A.mark()
        hT, hTb = A.alloc([16, 1024], BF16, nbufs=8, name="hT")
        phase_norm(C, lambda gi: out.ap()[gi * 128:(gi + 1) * 128, :], lambda gi: outb[gi], R_N1, tiles, hT, hTb)
        slabs = SlabRing(A, 4, 16, 256, "fslab")
        sgr = Ring(A, 4, [1024], F32, "sg")
        rhs = lambda kc, g, hT=hT: hT[:, kc, g * 512:(g + 1) * 512]
        for s in range(22):
            s1 = slabs.next()
            b1 = load_slab(C, s1, w1, 0, 16, s * 256, 256)
            s3 = slabs.next()
            b3 = load_slab(C, s3, w3, 0, 16, s * 256, 256)
            for jj in range(2):
                fb = s * 2 + jj
                bk1 = rr(C, "ffpair", [0, 2, 4, 6])
                bk3 = rr(C, "ffpair", [0, 2, 4, 6])
                mm_unitB(C, bk1, s1[0], b1, jj * 128, 16, rhs, hTb)
                mm_unitB(C, bk3, s3[0], b3, jj * 128, 16, rhs, hTb)
                sg, sgb = sgr.next()
                P.op("act", lambda e, sg=sg, bk1=bk1: e.activation(sg, flat2(C, bk1), AF.Silu),
                     reads=[C.bank[bk1], C.bank[bk1 + 1]], writes=[sgb])
                P.op("dve", lambda e, sg=sg, bk3=bk3, fb=fb: e.tensor_tensor(hid[:, fb, :], sg, flat2(C, bk3), ALU.mult),
                     reads=[sgb, C.bank[bk3], C.bank[bk3 + 1]], writes=[hidb[fb]])
        A.release(m2)
        phase_outproj(C, "w2", [5632, 2048], 44, lambda kc, li, hid=hid: hid[:, kc, li * 128:(li + 1) * 128], lambda li, hidb=hidb: hidb, tiles,
                      lambda gi, cg: out_tile_ap(out, gi, cg * 512, 512), lambda gi, cg: [outb[gi][cg]],
                      lambda gi, cg: out_tile_ap(out, gi, cg * 512, 512), lambda gi, cg: [outb[gi][cg]], kslab=22)
        A.release(m)


def gelu_chain(C, xt, xb, t1, t1b, sgm, sgmb, shape_free):
    P = C.P
    P.op("act", lambda e: e.activation(t1, xt, AF.Square), reads=[xb], writes=[t1b])
    P.op("dve", lambda e: e.tensor_scalar(t1, t1, GELU_C, 1.0, ALU.mult, ALU.add), reads=[t1b], writes=[t1b])
    P.op("dve", lambda e: e.tensor_tensor(t1, t1, xt, ALU.mult), reads=[t1b, xb], writes=[t1b])
    P.op("act", lambda e: e.activation(sgm, t1, AF.Sigmoid, scale=GELU_K), reads=[t1b], writes=[sgmb])


def layer1_mixer(C, out, outb):
    A, P, ps, nc = C.A, C.P, C.ps, C.nc
    C.accd = nc.dram_tensor("accd", [2048 + CAP, 2048], F32)
    C.accb = [[Buf("acc%d_%d" % (i, j)) for j in range(4)] for i in range(16)]
    w_u = wview(din(C, "w_u", [2048, 4096]).ap())
    m0 = A.mark()
    wsTb, wsTbb = A.alloc([16, 128], BF16, name="wsTb")
    bsbc, bsbcb = A.alloc([16, 128], F32, name="bsbc")
    mm_ = A.mark()
    wsf, wsfb = A.alloc([16, 128], F32, name="wsf")
    P.dma("sp", wsf, din(C, "wsT", [128, 16, 128]).ap(), writes=[wsfb])
    P.op("pool", lambda e: e.affine_select(wsf, wsf, [[0, 16], [1, 128]], ALU.is_ge, 0.0, base=0, channel_multiplier=-1),
         reads=[wsfb], writes=[wsfb])
    P.op("dve", lambda e: e.tensor_copy(wsTb, wsf), reads=[wsfb], writes=[wsTbb])
    A.release(mm_)
    P.dma("sp", bsbc.rearrange("p a b -> p (a b)"), row_bcast(C, R_BS), writes=[bsbcb])
    for h in range(2):
        tiles = list(range(h * 8, h * 8 + 8))
        m = A.mark()
        gT, gTb = A.alloc([16, 1024], BF16, nbufs=16, name="gT")
        vln, vlnb = A.alloc([8, 2048], BF16, nbufs=8, name="vln")
        m2 = A.mark()
        hT, hTb = A.alloc([16, 1024], BF16, nbufs=8, name="hT")
        phase_norm(C, lambda gi: out.ap()[gi * 128:(gi + 1) * 128, :], lambda gi: outb[gi], R_N2, tiles, hT, hTb)
        m3 = A.mark()
        bubc, bubcb = A.alloc([2048], F32, name="bubc")
        P.dma("sp", bubc, row_bcast(C, R_BU2), writes=[bubcb])
        s1, s1b = A.alloc([8, 4], F32, nbufs=8, name="s1")
        s2, s2b = A.alloc([8, 4], F32, nbufs=8, name="s2")
        m4 = A.mark()
        slabs = SlabRing(A, 2, 16, 512, "vslab")
        ztr = Ring(A, 4, [512], F32, "zt")
        t1r = Ring(A, 4, [512], F32, "t1")
        sgr = Ring(A, 4, [512], F32, "sgm")
        vtr = Ring(A, 4, [512], F32, "vt")
        junk, junkb = A.alloc([512], F32, name="junk")
        for cg in range(4):
            sl = slabs.next()
            sb = load_slab(C, sl, w_u, 0, 16, 2048 + cg * 512, 512)
            for li in range(8):
                bk = rr(C, "gvbank", [0, 1, 2, 3, 4, 5])
                mm_unitA(C, bk, lambda kc, li=li, hT=hT: hT[:, kc, li * 128:(li + 1) * 128], [hTb[li]], sl[0], sb, 16, 512)
                zt, ztb = ztr.next()
                t1, t1b = t1r.next()
                sgm, sgmb = sgr.next()
                vt, vtb = vtr.next()
                P.op("dve", lambda e, zt=zt, bk=bk, cg=cg: e.tensor_tensor(zt, ps[:, bk, :], bubc[:, cg * 512:(cg + 1) * 512], ALU.add),
                     reads=[C.bank[bk], bubcb], writes=[ztb])
                gelu_chain(C, zt, ztb, t1, t1b, sgm, sgmb, None)
                P.op("dve", lambda e, zt=zt, sgm=sgm, vt=vt, li=li, cg=cg: e.scalar_tensor_tensor(vt, zt, 1.0, sgm, ALU.mult, ALU.mult, accum_out=s1[:, li, cg:cg + 1]),
                     reads=[ztb, sgmb], writes=[vtb, s1b[li]])
                P.op("act", lambda e, vt=vt, li=li, cg=cg: e.activation(junk, vt, AF.Square, accum_out=s2[:, li, cg:cg + 1]),
                     reads=[vtb], writes=[junkb, s2b[li]])
                P.op("act", lambda e, vt=vt, li=li, cg=cg: e.activation(vln[:, li, cg * 512:(cg + 1) * 512], vt, AF.Copy),
                     reads=[vtb], writes=[vlnb[li]])
        A.release(m4)
        vgbc, vgbcb = A.alloc([2048], F32, name="vgbc")
        vbbc, vbbcb = A.alloc([2048], F32, name="vbbc")
        P.dma("sp", vgbc, row_bcast(C, R_VNG), writes=[vgbcb])
        P.dma("sp", vbbc, row_bcast(C, R_VNB), writes=[vbbcb])
        st_, stb = A.alloc([8, 4], F32, nbufs=8, name="lnst")
        lnr = Ring(A, 2, [2048], F32, "lnt")
        for li in range(8):
            mean, var, rstd, sm2 = (st_[:, li, k:k + 1] for k in range(4))
            P.op("dve", lambda e, li=li, mean=mean: e.tensor_reduce(mean, s1[:, li, :], mybir.AxisListType.X, ALU.add), reads=[s1b[li]], writes=[stb[li]])
            P.op("dve", lambda e, li=li, sm2=sm2: e.tensor_reduce(sm2, s2[:, li, :], mybir.AxisListType.X, ALU.add), reads=[s2b[li]], writes=[stb[li]])
            P.op("dve", lambda e, mean=mean: e.tensor_scalar(mean, mean, 1.0 / 2048, None, ALU.mult), reads=[stb[li]], writes=[stb[li]])
            P.op("dve", lambda e, mean=mean, var=var: e.tensor_tensor(var, mean, mean, ALU.mult), reads=[stb[li]], writes=[stb[li]])
            P.op("dve", lambda e, var=var, sm2=sm2: e.scalar_tensor_tensor(var, sm2, 1.0 / 2048, var, ALU.mult, ALU.subtract), reads=[stb[li]], writes=[stb[li]])
            P.op("act", lambda e, var=var, rstd=rstd: e.activation(rstd, var, AF.Sqrt, bias=EPS), reads=[stb[li]], writes=[stb[li]])
            P.op("dve", lambda e, rstd=rstd: e.reciprocal(rstd, rstd), reads=[stb[li]], writes=[stb[li]])
            lt, ltb = lnr.next()
            P.op("dve", lambda e, lt=lt, li=li, mean=mean, rstd=rstd: e.tensor_scalar(lt, vln[:, li, :], mean, rstd, ALU.subtract, ALU.mult),
                 reads=[vlnb[li], stb[li]], writes=[ltb])
            P.op("dve", lambda e, lt=lt: e.tensor_tensor(lt, lt, vgbc, ALU.mult), reads=[vgbcb], writes=[ltb])
            P.op("dve", lambda e, lt=lt, li=li: e.tensor_tensor(vln[:, li, :], lt, vbbc, ALU.add), reads=[ltb, vbbcb], writes=[vlnb[li]])
        A.release(m3)
        slabs = SlabRing(A, 3, 16, 256, "uslab")
        xr = Ring(A, 3, [1024], F32, "xu")
        t1r = Ring(A, 3, [1024], F32, "t1u")
        sgr = Ring(A, 3, [1024], F32, "sgu")
        tmr = Ring(A, 3, [1024], F32, "tmu")
        rhs = lambda kc, g, hT=hT: hT[:, kc, g * 512:(g + 1) * 512]
        for s in range(8):
            sl = slabs.next()
            sb = load_slab(C, sl, w_u, 0, 16, s * 256, 256)
            for jj in range(2):
                g = s * 2 + jj
                bk = rr(C, "gupair", [0, 2])
                mm_unitB(C, bk, sl[0], sb, jj * 128, 16, rhs, hTb)
                xt, xb = xr.next()
                t1, t1b = t1r.next()
                sgm, sgmb = sgr.next()
                tm, tmb = tmr.next()
                P.op("act", lambda e, xt=xt, bk=bk, g=g: e.activation(xt, flat2(C, bk), AF.Identity, bias=C.pp[:, PP_BU + g:PP_BU + g + 1]),
                     reads=[C.bank[bk], C.bank[bk + 1], C.b_pp], writes=[xb])
                gelu_chain(C, xt, xb, t1, t1b, sgm, sgmb, None)
                P.op("dve", lambda e, xt=xt, sgm=sgm: e.tensor_tensor(xt, xt, sgm, ALU.mult), reads=[sgmb], writes=[xb])
                gb_ = rr(C, "ggpair", [4, 6])

                def gate(e, g=g, gb_=gb_, vln=vln):
                    for li in range(8):
                        ins = e.matmul(ps[:, gb_ + li // 4, (li % 4) * 128:(li % 4 + 1) * 128], vln[:, li, g * 128:(g + 1) * 128], wsTb[:, g, :],
                                       start=True, stop=True)
                    return ins
                P.op("pe", gate, reads=vlnb + [wsTbb], writes=[C.bank[gb_], C.bank[gb_ + 1]])
                a0 = bsbc[:, g, :]
                zap = bass.AP(a0.tensor, a0.offset, [list(a0.ap[0]), [0, 8], [1, 128]])
                P.op("dve", lambda e, tm=tm, gb_=gb_, zap=zap: e.tensor_tensor(tm.rearrange("p (a b) -> p a b", a=8), flat2(C, gb_).rearrange("p (a b) -> p a b", a=8), zap, ALU.add),
                     reads=[C.bank[gb_], C.bank[gb_ + 1], bsbcb], writes=[tmb])
                P.op("dve", lambda e, tm=tm, xt=xt, g=g: e.tensor_tensor(gT[:, g, :], tm, xt, ALU.mult), reads=[tmb, xb], writes=[gTb[g]])
        A.release(m2)
        w_o_name = "w_o"
        phase_outproj(C, w_o_name, [2048, 2048], 16, lambda kc, li, gT=gT: gT[:, kc, li * 128:(li + 1) * 128], lambda li, gTb=gTb: gTb, tiles,
                      lambda gi, cg: out_tile_ap(out, gi, cg * 512, 512), lambda gi, cg: [outb[gi][cg]],
                      lambda gi, cg: out_tile_ap(C.accd, gi, cg * 512, 512), lambda gi, cg: [C.accb[gi][cg]])
        A.release(m)
    A.release(m0)


def zbc(ap3, n):
    return bass.AP(ap3.tensor, ap3.offset, [list(ap3.ap[0]), list(ap3.ap[1]), [0, n]])


def load_slab_c(C, slot, src2d, kcn, w):
    ap, bufs = slot
    i = 0
    for k in range(0, kcn, 8):
        k2 = min(k + 8, kcn)
        C.P.dma("pool", ap[:, k:k2, 0:w].rearrange("p a b -> p (a b)"), src2d[:, k * w:k2 * w], writes=[bufs[i]])
        i += 1
    return bufs[:i]


class WStream:
    def __init__(self, C, ring, loads):
        self.C, self.ring, self.loads = C, ring, loads
        self.n = len(ring.slots)
        self.issued = []
        self.next_use = 0
        for _ in range(self.n):
            self._issue()

    def _issue(self):
        i = len(self.issued)
        if i >= len(self.loads):
            return
        slot = self.ring.slots[i % self.n]
        bufs = load_slab_c(self.C, slot, *self.loads[i])
        self.issued.append((slot[0], bufs))

    def get(self):
        r = self.issued[self.next_use]
        self.next_use += 1
        return r

    def done(self):
        self._issue()


def layer1_moe(C, out, outb):
    A, P, ps, nc = C.A, C.P, C.ps, C.nc
    H = CAPU // 2
    m0 = A.mark()
    lg, lgb = A.alloc([16, 8], F32, nbufs=16, name="lg")
    top8, top8b = A.alloc([16, 8], F32, name="top8")
    Mk, Mkb_ = A.alloc([16, 8], F32, name="Mk")
    Gt, Gtb = A.alloc([16, 8], F32, name="Gt")
    excl, exclb = A.alloc([16, 8], F32, name="excl")
    exclm, exclmb = A.alloc([16, 8], F32, name="exclm")
    Mkh, Mkhb = A.alloc([16, 8], BF16, name="Mkh")
    gsc, gscb = A.alloc([16, 4], F32, name="gsc")
    I32 = mybir.dt.int32
    accd, accb = C.accd, C.accb
    accall = Buf("accall")
    gsl, gslb = A.alloc([8, NSL], F32, name="gsl")
    idxs, idxsb = A.alloc([8, 8], I32, name="idxs")
    hted = nc.dram_tensor("hted", [8, 128, 16 * CAPU], BF16)
    htedb = [Buf("hted%d" % e) for e in range(8)]
    m1 = A.mark()
    htokd = nc.dram_tensor("htokd", [2048, 2048], BF16)
    htokdb = [Buf("htokd%d" % i) for i in range(16)]
    m1b = A.mark()
    gw, gwb = A.alloc([16, 8], F32, name="gw")
    P.dma("sp", gw, din(C, "wrfm", [128, 16, 8]).ap(), writes=[gwb])
    a0 = C.pp[:, PP_N3:PP_N3 + 16]
    gb3 = bass.AP(a0.tensor, a0.offset, [list(a0.ap[0]), [1, 16], [0, 8]])
    P.op("dve", lambda e: e.tensor_tensor(gw, gw, gb3, ALU.mult), reads=[C.b_pp], writes=[gwb])
    xTr = Ring(A, 2, [16, 128], F32, "xT", nbufs=4)

    def post(li, gi, xt, xb, rs, rsb, gbc, gb):
        xTt, xTb = xTr.next()
        for j in range(4):
            bk = rr(C, "rtbank", [0, 1, 2, 3, 4, 5])

            def tr(e, j=j, bk=bk, xt=xt):
                for k in range(4):
                    kc = j * 4 + k
                    ins = e.transpose(ps[:, bk, k * 128:(k + 1) * 128], xt[:, kc * 128:(kc + 1) * 128], C.identf)
                return ins
            P.op("pe", tr, reads=[xb, C.b_identf], writes=[C.bank[bk]])
            dst = xTt[:, j * 4:(j + 1) * 4, :]
            src = ps[:, bk, :].rearrange("p (k t) -> p k t", k=4)
            if j % 2 == 0:
                P.op("act", lambda e, dst=dst, src=src: e.activation(dst, src, AF.Copy), reads=[C.bank[bk]], writes=[xTb[j]])
            else:
                P.op("dve", lambda e, dst=dst, src=src: e.tensor_copy(dst, src), reads=[C.bank[bk]], writes=[xTb[j]])
        lb_ = rr(C, "lgbank", [6, 7])

        def rt(e, xTt=xTt, lb_=lb_):
            for kc in range(16):
                ins = e.matmul(ps[:, lb_, 0:8], xTt[:, kc, :], gw[:, kc, :], start=(kc == 0), stop=(kc == 15))
            return ins
        P.op("pe", rt, reads=xTb + [gwb], writes=[C.bank[lb_]])
        P.op("act", lambda e, li=li, lb_=lb_, rs=rs: e.activation(lg[:, li, :], ps[:, lb_, 0:8], AF.Copy, scale=rs), reads=[C.bank[lb_], rsb], writes=[lgb[li]])
    def tokout(li, gi, hb, hbb):
        P.dma("sp", htokd.ap()[gi * 128:(gi + 1) * 128, :], hb, reads=[hbb], writes=[htokdb[gi]])
    phase_norm(C, lambda gi: accd.ap()[gi * 128:(gi + 1) * 128, :], lambda gi: accb[gi], R_N3, list(range(16)), None, None,
               post=post, tokout=tokout)
    A.release(m1b)
    for li in range(16):
        P.op("dve", lambda e, li=li: e.max(top8[:, li, :], lg[:, li, :]), reads=[lgb[li]], writes=[top8b])
    m1v = zbc(top8[:, :, 0:1], 8)
    m2v = zbc(top8[:, :, 1:2], 8)
    mtmp, mtmpb = A.alloc([16, 8], F32, name="mtmp")
    P.op("dve", lambda e: e.tensor_tensor(gsc[:, :, 2:3], top8[:, :, 0:1], top8[:, :, 1:2], ALU.subtract), reads=[top8b], writes=[gscb])
    P.op("act", lambda e: e.activation(gsc[:, :, 0:1], gsc[:, :, 2:3], AF.Sigmoid), reads=[gscb], writes=[gscb])
    P.op("dve", lambda e: e.tensor_scalar(gsc[:, :, 1:2], gsc[:, :, 0:1], -1.0, 1.0, ALU.mult, ALU.add), reads=[gscb], writes=[gscb])
    P.op("dve", lambda e: e.tensor_tensor(Mk, lg, m1v, ALU.is_equal), reads=lgb + [top8b], writes=[Mkb_])
    P.op("dve", lambda e: e.tensor_tensor(mtmp, lg, m2v, ALU.is_equal), reads=lgb + [top8b], writes=[mtmpb])
    P.op("dve", lambda e: e.tensor_tensor(Gt, Mk, zbc(gsc[:, :, 0:1], 8), ALU.mult), reads=[Mkb_, gscb], writes=[Gtb])
    P.op("dve", lambda e: e.tensor_tensor(exclm, mtmp, zbc(gsc[:, :, 1:2], 8), ALU.mult), reads=[mtmpb, gscb], writes=[exclmb])
    P.op("dve", lambda e: e.tensor_tensor(Gt, Gt, exclm, ALU.add), reads=[exclmb], writes=[Gtb])
    P.op("dve", lambda e: e.tensor_tensor(Mk, Mk, mtmp, ALU.add), reads=[mtmpb], writes=[Mkb_])
    P.op("dve", lambda e: e.tensor_copy(Mkh, Mk), reads=[Mkb_], writes=[Mkhb])
    for li in range(16):
        bk = rr(C, "pfbank", [0, 1, 2, 3])

        def pf(e, li=li, bk=bk):
            for i in range(li + 1):
                lhs = C.onesb if i < li else C.triub
                ins = e.matmul(ps[:, bk, 0:8], lhs, Mkh[:, i, :], start=(i == 0), stop=(i == li))
            return ins
        P.op("pe", pf, reads=[Mkhb, C.b_onesb, C.b_triub], writes=[C.bank[bk]])
        P.op("act", lambda e, li=li, bk=bk: e.activation(excl[:, li, :], ps[:, bk, 0:8], AF.Copy), reads=[C.bank[bk]], writes=[exclb])
    P.op("dve", lambda e: e.scalar_tensor_tensor(exclm, excl, 1.0, Mk, ALU.add, ALU.mult), reads=[exclb, Mkb_], writes=[exclmb])
    P.op("dve", lambda e: e.tensor_scalar(exclm, exclm, -1.0, None, ALU.add), reads=[exclmb], writes=[exclmb])
    I32 = mybir.dt.int32
    tkf, tkfb = A.alloc([16, 3], F32, name="tkf")
    P.op("pool", lambda e: e.iota(tkf[:, :, 0], [[0, 16]], base=0, channel_multiplier=1, allow_small_or_imprecise_dtypes=True), writes=[tkfb])
    P.op("pool", lambda e: e.iota(tkf[:, :, 1], [[1, 16]], base=0, channel_multiplier=0, allow_small_or_imprecise_dtypes=True), writes=[tkfb])
    P.op("pool", lambda e: e.memset(tkf[:, :, 2], 1.0), writes=[tkfb])
    Gh, Ghb = A.alloc([16, 8], BF16, name="Gh")
    Gm, Gmb = A.alloc([16, 8], BF16, name="Gm")
    Gl, Glb = A.alloc([16, 8], BF16, name="Gl")
    gr, grb = A.alloc([16, 8], F32, name="gr")
    gr2, gr2b = A.alloc([16, 8], F32, name="gr2")
    P.op("dve", lambda e: e.tensor_copy(Gh, Gt), reads=[Gtb], writes=[Ghb])
    P.op("dve", lambda e: e.tensor_copy(gr2, Gh), reads=[Ghb], writes=[gr2b])
    P.op("dve", lambda e: e.tensor_tensor(gr, Gt, gr2, ALU.subtract), reads=[Gtb, gr2b], writes=[grb])
    P.op("dve", lambda e: e.tensor_copy(Gm, gr), reads=[grb], writes=[Gmb])
    P.op("dve", lambda e: e.tensor_copy(gr2, Gm), reads=[Gmb], writes=[gr2b])
    P.op("dve", lambda e: e.tensor_tensor(gr, gr, gr2, ALU.subtract), reads=[gr2b], writes=[grb])
    P.op("dve", lambda e: e.tensor_copy(Gl, gr), reads=[grb], writes=[Glb])
    tkr = Ring(A, 3, [16, 6], BF16, "tk6")
    selr = Ring(A, 3, [16, CAPU], BF16, "sel", nbufs=16)
    hslr = Ring(A, 3, [NSL, 2048], BF16, "hslot", nbufs=NSL)
    hter = Ring(A, 2, [16, CAPU], BF16, "hte", nbufs=16)
    idr = Ring(A, 3, [48], F32, "idxf")
    idir = Ring(A, 3, [8], I32, "idxi")
    for e_ in range(8):
        sel, selb = selr.next()
        for li in range(16):
            P.op("dve", lambda e, sel=sel, li=li, e_=e_: e.tensor_scalar(sel[:, li, :], C.iorow[:, 0:CAPU], excl[:, li, e_:e_ + 1], Mk[:, li, e_:e_ + 1], ALU.is_equal, ALU.mult),
                 reads=[C.b_iorow, exclb, Mkb_], writes=[selb[li]])
        tk6, tk6b = tkr.next()
        P.op("dve", lambda e, tk6=tk6: e.tensor_copy(tk6[:, :, 0:3], tkf), reads=[tkfb], writes=[tk6b])
        for k_, (gt_, gtb_) in enumerate(((Gh, Ghb), (Gm, Gmb), (Gl, Glb))):
            P.op("dve", lambda e, tk6=tk6, gt_=gt_, k_=k_, e_=e_: e.tensor_copy(tk6[:, :, 3 + k_], gt_[:, :, e_]), reads=[gtb_], writes=[tk6b])
        bk = rr(C, "ixbank", [6, 7])

        def ix(e, sel=sel, bk=bk, tk6=tk6):
            for sl in range(NSL):
                for li in range(16):
                    ins = e.matmul(ps[:, bk, sl * 6:sl * 6 + 6], sel[:, li, sl * 128:(sl + 1) * 128], tk6[:, li, :], start=(li == 0), stop=(li == 15))
            return ins
        P.op("pe", ix, reads=selb + [tk6b], writes=[C.bank[bk]])
        idf, idfb = idr.next()
        idi, idib = idir.next()
        P.op("act", lambda e, idf=idf, bk=bk: e.activation(idf[:, 0:6 * NSL], ps[:, bk, 0:6 * NSL], AF.Copy), reads=[C.bank[bk]], writes=[idfb])
        idv = idf[:, 0:6 * NSL].rearrange("p (s c) -> p s c", c=6)
        tix = idf[:, 32:32 + NSL]
        tsc = idf[:, 40:40 + NSL]
        P.op("dve", lambda e, idv=idv, tix=tix: e.scalar_tensor_tensor(tix, idv[:, :, 1], 128.0, idv[:, :, 0], ALU.mult, ALU.add),
             reads=[idfb], writes=[idfb])
        P.op("dve", lambda e, idi=idi, tix=tix: e.tensor_copy(idi[:, 0:NSL], tix), reads=[idfb], writes=[idib])
        P.op("dve", lambda e, idv=idv, e_=e_: e.tensor_tensor(gsl[:, e_, :], idv[:, :, 3], idv[:, :, 4], ALU.add), reads=[idfb], writes=[gslb])
        P.op("dve", lambda e, idv=idv, e_=e_: e.tensor_tensor(gsl[:, e_, :], gsl[:, e_, :], idv[:, :, 5], ALU.add), reads=[idfb], writes=[gslb])
        P.op("dve", lambda e, tsc=tsc, tix=tix: e.tensor_tensor(tsc, tix, C.iops, ALU.subtract), reads=[idfb, C.b_iops], writes=[idfb])
        P.op("dve", lambda e, tsc=tsc: e.tensor_scalar(tsc, tsc, -2048.0, None, ALU.add), reads=[idfb], writes=[idfb])
        P.op("dve", lambda e, tsc=tsc, idv=idv: e.tensor_tensor(tsc, tsc, idv[:, :, 2], ALU.mult), reads=[idfb], writes=[idfb])
        P.op("dve", lambda e, tsc=tsc: e.tensor_tensor(tsc, tsc, C.iops, ALU.add), reads=[idfb, C.b_iops], writes=[idfb])
        P.op("dve", lambda e, tsc=tsc: e.tensor_scalar(tsc, tsc, 2048.0, None, ALU.add), reads=[idfb], writes=[idfb])
        P.op("dve", lambda e, tsc=tsc, e_=e_: e.tensor_copy(idxs[:, e_, 0:NSL], tsc), reads=[idfb], writes=[idxsb])
        hsl, hslb = hslr.next()
        for sl in range(NSL):
            deps = P._deps([idib] + htokdb, [hslb[sl]])
            i_ = P.dma_i["pool"]
            P.dma_i["pool"] += 1
            k_ = "d_pool_%d" % (i_ % P.NDMA)
            if P.cnt[k_] > 0:
                deps.append((k_, P.cnt[k_]))
            P._need("pool", deps)
            P.cnt[k_] += 16
            sem_ = P.sems[k_]
            P.q["pool"].append(lambda e, sl=sl, sem_=sem_, hsl=hsl, idi=idi: e.indirect_dma_start(
                out=hsl[:, sl, :], out_offset=None, in_=htokd.ap(),
                in_offset=bass.IndirectOffsetOnAxis(ap=idi[:, sl:sl + 1], axis=0)).then_inc(sem_, 16))
            P.nins["pool"] += 1
            P._commit((k_, P.cnt[k_]), [idib] + htokdb, [hslb[sl]])
        hte, hteb = hter.next()
        for fb in range(16):
            bk2 = rr(C, "gtbank", [0, 1, 2, 3, 4, 5])
            pb = ps[:, bk2, :].bitcast(BF16)

            def tr(e, hsl=hsl, fb=fb, pb=pb):
                for sl in range(NSL):
                    ins = e.transpose(pb[:, sl * 128:(sl + 1) * 128], hsl[:, sl, fb * 128:(fb + 1) * 128], C.identb)
                return ins
            P.op("pe", tr, reads=hslb + [C.b_identb], writes=[C.bank[bk2]])
            if fb % 2 == 0:
                P.op("act", lambda e, hte=hte, fb=fb, pb=pb: e.activation(hte[:, fb, :], pb[:, 0:CAPU], AF.Copy), reads=[C.bank[bk2]], writes=[hteb[fb]])
            else:
                P.op("dve", lambda e, hte=hte, fb=fb, pb=pb: e.tensor_copy(hte[:, fb, :], pb[:, 0:CAPU]), reads=[C.bank[bk2]], writes=[hteb[fb]])
        P.dma("sp", hted.ap()[e_], hte.rearrange("p a b -> p (a b)"), reads=hteb, writes=[htedb[e_]])
    A.release(m1)
    wg_all = din(C, "wg", [8, 28, 128, 4096]).ap()
    wu_all = din(C, "wu", [8, 28, 128, 4096]).ap()
    wd_all = din(C, "wd", [8, 16, 128, 7168]).ap()
    NPRE = 4
    gl, ul, dl = [], [], []
    for e_ in range(8):
        for q in range(4):
            for s_ in range(7):
                gl.append((wg_all[e_][q * 7 + s_], 16, 256))
                ul.append((wu_all[e_][q * 7 + s_], 16, 256))
            for cg in range(4):
                dl.append((wd_all[e_][q * 4 + cg], 14, 512))
    gst = WStream(C, SlabRing(A, 2, 16, 256, "gslab"), gl)
    ust = WStream(C, SlabRing(A, 2, 16, 256, "uslab"), ul)
    dst_ = WStream(C, SlabRing(A, 2, 14, 512, "dslab"), dl)
    hTer = Ring(A, 2, [16, CAPU], BF16, "hTe")
    hTes = {}

    def load_hTe(e_):
        hTe, hTeb = hTer.next()
        P.dma("sp", hTe.rearrange("p a b -> p (a b)"), hted.ap()[e_], reads=[htedb[e_]], writes=[hTeb])
        hTes[e_] = (hTe, hTeb)
    load_hTe(0)
    for e_ in range(8):
        me = A.mark()
        hTe, hTeb = hTes[e_]
        if e_ + 1 < 8:
            load_hTe(e_ + 1)
        y, yb_ = A.alloc([NSL, 2048], F32, nbufs=NSL * 4, name="y")
        ybuf = lambda sl, cg: yb_[sl * 4 + cg]
        mf = A.mark()
        hidr = Ring(A, 1, [14, CAP], BF16, "hidq", nbufs=14)
        sgr = Ring(A, 2, [2, H], F32, "esg")
        rhs = lambda kc, g, hTe=hTe: hTe[:, kc, g * H:(g + 1) * H]
        for q in range(4):
            hidq, hidqb = hidr.next()
            if CAPU < CAP and q == 0:
                P.op("dve", lambda e, hidq=hidq: e.memset(hidq[:, :, CAPU:CAP], 0.0), writes=hidqb)
            for s in range(7):
                sg_ = gst.get()
                bg = sg_[1]
                su_ = ust.get()
                bu = su_[1]
                for jj in range(2):
                    j = s * 2 + jj
                    bkg = rr(C, "epair", [0, 2, 4, 6])
                    bku = rr(C, "epair", [0, 2, 4, 6])

                    def up(e, slab, bk, jj=jj, rhs=rhs):
                        for kc in range(16):
                            for g in range(2):
                                ins = e.matmul(ps[:, bk + g, 0:H], slab[:, kc, jj * 128:(jj + 1) * 128], rhs(kc, g), start=(kc == 0), stop=(kc == 15))
                        return ins
                    P.op("pe", lambda e, up=up, slab=sg_[0], bk=bkg: up(e, slab, bk), reads=bg + [hTeb], writes=[C.bank[bkg], C.bank[bkg + 1]])
                    P.op("pe", lambda e, up=up, slab=su_[0], bk=bku: up(e, slab, bk), reads=bu + [hTeb], writes=[C.bank[bku], C.bank[bku + 1]])
                    sg, sgb = sgr.next()
                    P.op("act", lambda e, sg=sg, bkg=bkg: e.activation(sg, ps[:, bkg:bkg + 2, 0:H], AF.Silu), reads=[C.bank[bkg], C.bank[bkg + 1]], writes=[sgb])
                    P.op("dve", lambda e, sg=sg, bku=bku, j=j, hidq=hidq: e.tensor_tensor(hidq[:, j, 0:CAPU].rearrange("p (a b) -> p a b", a=2), sg, ps[:, bku:bku + 2, 0:H], ALU.mult),
                         reads=[sgb, C.bank[bku], C.bank[bku + 1]], writes=[hidqb[j]])
                gst.done()
                ust.done()
            for cg in range(4):
                dsl = dst_.get()
                bd = dsl[1]
                for sl in range(NSL):
                    bk = rr(C, "edbank", [0, 1, 2, 3, 4, 5, 6, 7])
                    mm_unitA(C, bk, lambda kc, sl=sl, hidq=hidq: hidq[:, kc, sl * 128:(sl + 1) * 128], hidqb, dsl[0], bd, 14, 512)
                    yv = y[:, sl, cg * 512:(cg + 1) * 512]
                    if q == 0:
                        P.op("act", lambda e, yv=yv, bk=bk: e.activation(yv, ps[:, bk, :], AF.Copy), reads=[C.bank[bk]], writes=[ybuf(sl, cg)])
                    else:
                        P.op("dve", lambda e, yv=yv, bk=bk: e.tensor_tensor(yv, yv, ps[:, bk, :], ALU.add), reads=[C.bank[bk]], writes=[ybuf(sl, cg)])
                dst_.done()
        A.release(mf)
        for sl in range(NSL):
            yrow = y[:, sl, :]
            yb4 = [ybuf(sl, c) for c in range(4)]
            if sl % 2 == 0:
                P.op("act", lambda e, yrow=yrow, sl=sl, e_=e_: e.activation(yrow, yrow, AF.Copy, scale=gsl[:, e_, sl:sl + 1]), reads=[gslb], writes=yb4)
            else:
                P.op("dve", lambda e, yrow=yrow, sl=sl, e_=e_: e.tensor_scalar(yrow, yrow, gsl[:, e_, sl:sl + 1], None, ALU.mult), reads=[gslb], writes=yb4)
            wr = [accall] + ([b_ for row in accb for b_ in row] if (e_ == 0 and sl == 0) else [])
            deps = P._deps(yb4 + [idxsb], wr)
            i_ = P.dma_i["pool"]
            P.dma_i["pool"] += 1
            k_ = "d_pool_%d" % (i_ % P.NDMA)
            if P.cnt[k_] > 0:
                deps.append((k_, P.cnt[k_]))
            P._need("pool", deps)
            P.cnt[k_] += 16
            sem_ = P.sems[k_]
            P.q["pool"].append(lambda e, yrow=yrow, sl=sl, e_=e_, sem_=sem_: e.indirect_dma_start(
                out=accd.ap(), out_offset=bass.IndirectOffsetOnAxis(ap=idxs[:, e_, sl:sl + 1], axis=0),
                in_=yrow, in_offset=None, compute_op=ALU.add).then_inc(sem_, 16))
            P.nins["pool"] += 1
            P._commit((k_, P.cnt[k_]), yb4 + [idxsb], wr)
        A.release(me)
    for gi in range(16):
        P.dma("sp", out.ap()[gi * 128:(gi + 1) * 128, :], accd.ap()[gi * 128:(gi + 1) * 128, :], reads=[accall], writes=outb[gi])
    A.release(m0)

import math


def host_small(inp):
    f = lambda k: np.asarray(inp[k], np.float32)
    pp = np.zeros((128, NPP), np.float32)
    pp[:, PP_CONVB:PP_CONVB + 8] = f("even_conv_b")[0].reshape(8, 128).T
    pp[:, PP_CNG:PP_CNG + 8] = f("even_cnorm_g")[0].reshape(8, 128).T
    pp[:, PP_CNB:PP_CNB + 8] = f("even_cnorm_b")[0].reshape(8, 128).T
    pp[:, PP_BU:PP_BU + 16] = f("odd_b_u")[0][:2048].reshape(16, 128).T
    pp[:, PP_QG] = f("even_q_norm")[0]
    pp[:, PP_KG] = f("even_k_norm")[0]
    pp[:, PP_N3:PP_N3 + 16] = f("odd_norm_ffn")[0].reshape(16, 128).T
    cw = np.ascontiguousarray(f("even_conv_w")[0].T.reshape(8, 128, 31).transpose(1, 0, 2))
    rows = np.zeros((NROWS, 2048), np.float32)
    rows[R_N0] = f("even_norm_mix")[0]
    rows[R_N1] = f("even_norm_ffn")[0]
    rows[R_N2] = f("odd_norm_mix")[0]
    rows[R_N3] = f("odd_norm_ffn")[0]
    rows[R_BU2] = f("odd_b_u")[0][2048:]
    rows[R_VNG] = f("odd_vnorm_g")[0]
    rows[R_VNB] = f("odd_vnorm_b")[0]
    rows[R_BS] = f("odd_b_s")[0].reshape(-1)
    rows[R_ROUTER:R_ROUTER + 8] = f("odd_router")[0].T
    wsT = np.ascontiguousarray(f("odd_w_s")[0].transpose(2, 0, 1))
    wrfm = np.ascontiguousarray(f("odd_router")[0].reshape(16, 128, 8).transpose(1, 0, 2))
    return {"pp": pp, "cw": cw, "rows": rows, "wsT": wsT, "ohu": make_ohu(), "rel_bias": f("rel_bias"), "wrfm": wrfm}


def t5_bucket(d):
    max_exact = 16
    d = max(d, 0)
    if d < max_exact:
        return d
    lr = np.float32(np.log(np.float32(np.float32(max(d, 1)) / np.float32(max_exact)))) / np.float32(math.log(2048 / max_exact))
    large = max_exact + int(np.float32(np.float32(lr) * np.float32(32 - max_exact)))
    return min(large, 31)


def make_ohu():
    oh = np.zeros((64, 3 * 384), np.float32)
    for b, dil in enumerate(DILS):
        for mm in range(384):
            dm = mm - 127
            if 0 <= dm <= 128:
                oh[t5_bucket(dm * dil), b * 384 + mm] = 1.0
            else:
                oh[32, b * 384 + mm] = 1.0
    return oh


def build(upto=99, debug=False):
    nc = bass.Bass("TRN2", target_bir_lowering=False)
    st = ExitStack()
    with st:
        C = setup(nc, st)
        A, P = C.A, C.P
        x = din(C, "x", [2048, 2048])
        out = nc.dram_tensor("out", [2048, 2048], F32, kind="ExternalOutput")
        outb = [[Buf("out%d_%d" % (i, j)) for j in range(4)] for i in range(16)]
        xb_in = Buf("x")
        vd = nc.dram_tensor("vd", [2048, 1024], BF16)
        vdb = [Buf("vd%d" % i) for i in range(16)]
        hTd = nc.dram_tensor("hTd", [2, 128, 16 * 1024], BF16)
        hTdb = [Buf("hTd0"), Buf("hTd1")]
        consts(C)
        m0 = A.mark()
        ob, obb = A.alloc([8, 2048], BF16, nbufs=8, name="ob")
        tails, tailsb = A.alloc([8, 30], BF16, name="tails")
        x_ap = lambda gi: x.ap()[gi * 128:(gi + 1) * 128, :]
        x_bufs = lambda gi: [xb_in]
        for h in range(2):
            m = A.mark()
            hT, hTb = A.alloc([16, 1024], BF16, nbufs=8, name="hT")
            phase_norm(C, x_ap, x_bufs, R_N0, list(range(h * 8, h * 8 + 8)), hT, hTb)
            P.dma("sp", hTd.ap()[h], hT.rearrange("p a b -> p (a b)"), reads=hTb, writes=[hTdb[h]])
            phase_conv(C, h, hT, hTb, ob, obb, tails, tailsb)
            A.release(m)
        qkT, qkb = A.alloc([16, 2048], BF16, nbufs=16, name="qkT")
        for h in range(2):
            m = A.mark()
            hT, hTb1 = A.alloc([16, 1024], BF16, name="hT")
            hTb = [hTb1] * 8
            P.dma("sp", hT.rearrange("p a b -> p (a b)"), hTd.ap()[h], reads=[hTdb[h]], writes=[hTb1])
            phase_v(C, h, hT, hTb, vd, vdb)
            phase_qk(C, h, hT, hTb, qkT, qkb)
            A.release(m)
        m = A.mark()
        biasT, biasb = A.alloc([24, 256], BF16, name="biasT")
        phase_bias(C, biasT, biasb)
        phase_attn(C, qkT, qkb, vd, vdb, biasT, biasb)
        A.release(m)
        lhs = lambda kc, li: (qkT[:, kc, li * 128:(li + 1) * 128] if kc < 8 else ob[:, kc - 8, li * 128:(li + 1) * 128])
        lb = lambda li: qkb[0:8] + obb
        phase_outproj(C, "w_out", [2048, 2048], 16, lhs, lb, list(range(16)),
                      lambda gi, cg: x.ap()[gi * 128:(gi + 1) * 128, cg * 512:(cg + 1) * 512], lambda gi, cg: [xb_in],
                      lambda gi, cg: out.ap()[gi * 128:(gi + 1) * 128, cg * 512:(cg + 1) * 512], lambda gi, cg: [outb[gi][cg]])
        A.release(m0)
        if upto >= 2:
            layer0_ffn(C, out, outb)
        if upto >= 3:
            layer1_mixer(C, out, outb)
        if upto >= 4:
            layer1_moe(C, out, outb)
        P.wait_all("sp", [b for row in outb for b in row])
        print("arena peak", A.peak, "instr counts", P.nins, "sem counts", {k: v for k, v in P.cnt.items() if v})
        P.emit()
        nc._din_names = list(C.din.keys())
    return nc


_WMAP = {"w_in": "even_w_in", "w_out": "even_w_out", "w1": "even_ffn_w1", "w3": "even_ffn_w3", "w2": "even_ffn_w2",
         "w_u": "odd_w_u", "w_o": "odd_w_o"}


def tile_expert_weights(inputs):
    wg = np.asarray(inputs["odd_we_gate"], np.float32)[0]
    wu = np.asarray(inputs["odd_we_up"], np.float32)[0]
    wd = np.asarray(inputs["odd_we_down"], np.float32)[0]
    up = lambda w: np.ascontiguousarray(w.reshape(8, 16, 128, 28, 256).transpose(0, 3, 2, 1, 4)).reshape(8, 28, 128, 4096)
    dn = np.ascontiguousarray(wd.reshape(8, 4, 14, 128, 4, 512).transpose(0, 1, 4, 3, 2, 5)).reshape(8, 16, 128, 7168)
    return {"wg": up(wg), "wu": up(wu), "wd": dn}


def kernel(**inputs):
    n = 8
    nc = build(upto=4)
    small = host_small(inputs)
    shared = dict(small)
    for k, v in _WMAP.items():
        shared[k] = np.asarray(inputs[v], np.float32)[0]
    shared.update(tile_expert_weights(inputs))
    x = np.asarray(inputs["x"], np.float32)
    in_maps = []
    for c in range(n):
        m = {k: shared[k] for k in nc._din_names if k != "x"}
        m["x"] = x[c]
        in_maps.append({k: m[k] for k in nc._din_names})
    res = run_bass_kernel_spmd(nc, in_maps, core_ids=list(range(n)))
    return np.stack([res.results[c]["out"] for c in range(n)], axis=0).astype(np.float32)
```
